# Optimizing a Trainium2 kernel written in Bass

```python
import jax
import jax.numpy as jnp
from jax import lax
import numpy as np

D_MODEL = 2048
BATCH = 8
SEQ = 2048
DEPTH = 1

POOL_WINDOWS = (2, 4, 8, 16)
POOL_GROUPS = 4
POOL_GROUP_DIM = D_MODEL // 8
POOL_WIDTH = POOL_GROUPS * POOL_GROUP_DIM
N_HEADS = 16
N_KV_HEADS = 4
HEAD_DIM = 128
ATTN_WIDTH = N_HEADS * HEAD_DIM
KV_WIDTH = N_KV_HEADS * HEAD_DIM
ROT_DIM = HEAD_DIM // 4
IDX_HEADS = 16
IDX_DIM = 64
IDX_ROT_DIM = IDX_DIM // 4
TOPK_MAX = 256
Q_BLOCK = 64
ROPE_THETA = 500000.0
N_BRANCHES = 2
IN_SPLITS = (POOL_WIDTH, ATTN_WIDTH, KV_WIDTH, KV_WIDTH, IDX_HEADS * IDX_DIM, IDX_DIM, IDX_HEADS, N_BRANCHES * D_MODEL)
IN_WIDTH = POOL_WIDTH + ATTN_WIDTH + 2 * KV_WIDTH + IDX_HEADS * IDX_DIM + IDX_DIM + IDX_HEADS + N_BRANCHES * D_MODEL
N_EXPERTS = 32
TOP_K = 4
D_FF = D_MODEL
SWIGLU_ALPHA = 1.702
SWIGLU_LIMIT = 7.0
MOE_BLOCK = 256
N_MOD = 6
EPS = 1e-6

kernel_name = 'hybrid_pool_dsa_moe_block'


def rms_norm(x, g):
    xf = x.astype(jnp.float32)
    y = xf * lax.rsqrt(jnp.mean(xf * xf, axis=-1, keepdims=True) + EPS)
    return (y * g.astype(jnp.float32)).astype(x.dtype)


def rope_tables(seq, rot_dim):
    inv = ROPE_THETA ** (-jnp.arange(0, rot_dim, 2, dtype=jnp.float32) / rot_dim)
    ang = jnp.arange(seq, dtype=jnp.float32)[:, None] * inv[None, :]
    return jnp.cos(ang), jnp.sin(ang)


def partial_rope(x, cos, sin):
    half = cos.shape[-1]
    rot = 2 * half
    shape = (1, x.shape[1]) + (1,) * (x.ndim - 3) + (half,)
    c = cos.reshape(shape).astype(x.dtype)
    s = sin.reshape(shape).astype(x.dtype)
    x1 = x[..., :half]
    x2 = x[..., half:rot]
    return jnp.concatenate([x1 * c - x2 * s, x2 * c + x1 * s, x[..., rot:]], axis=-1)


def pool_mixer(u, w_grp, scale):
    b_, s_, _ = u.shape
    uf = u.astype(jnp.float32).reshape(b_, s_, POOL_GROUPS, POOL_GROUP_DIM)
    cs = jnp.concatenate([jnp.zeros_like(uf[:, :1]), jnp.cumsum(uf, axis=1)], axis=1)
    t = jnp.arange(s_)
    outs = []
    for g, w in enumerate(POOL_WINDOWS):
        csg = cs[:, :, g]
        lo = jnp.maximum(t + 1 - w, 0)
        cnt = jnp.minimum(t + 1, w).astype(jnp.float32)[None, :, None]
        outs.append((csg[:, t + 1] - csg[:, lo]) / cnt)
    pooled = jnp.stack(outs, axis=2)
    mixed = (pooled - uf).astype(u.dtype)
    y = jnp.einsum('bsgc,gcd->bsgd', mixed, w_grp).reshape(b_, s_, POOL_WIDTH)
    return y * scale


def dsa_attention(q, k, v, qi, ki, wi):
    b_, s_ = q.shape[0], q.shape[1]
    n_keep = min(TOPK_MAX, s_ // 4)
    n_blk = s_ // Q_BLOCK
    grp = N_HEADS // N_KV_HEADS
    gather = jax.vmap(lambda a, i: a[i])
    key_pos = jnp.arange(s_)

    def block(bi):
        start = bi * Q_BLOCK
        qb = lax.dynamic_slice_in_dim(q, start, Q_BLOCK, axis=1)
        qib = lax.dynamic_slice_in_dim(qi, start, Q_BLOCK, axis=1)
        wib = lax.dynamic_slice_in_dim(wi, start, Q_BLOCK, axis=1)
        t = start + jnp.arange(Q_BLOCK)
        causal = key_pos[None, :] <= t[:, None]
        dots = jnp.einsum('bqhd,bsd->bqhs', qib, ki, preferred_element_type=jnp.float32)
        score = jnp.einsum('bqhs,bqh->bqs', jax.nn.relu(dots), wib.astype(jnp.float32))
        score = jnp.where(causal[None], score, -jnp.inf)
        _, idx = lax.top_k(score, n_keep)
        valid = idx <= t[None, :, None]
        ks = gather(k, idx)
        vs = gather(v, idx)
        qg = qb.reshape(b_, Q_BLOCK, N_KV_HEADS, grp, HEAD_DIM)
        logits = jnp.einsum('bqgrd,bqkgd->bqgrk', qg, ks, preferred_element_type=jnp.float32) * (HEAD_DIM ** -0.5)
        logits = jnp.where(valid[:, :, None, None, :], logits, -jnp.inf)
        p = jax.nn.softmax(logits, axis=-1)
        o = jnp.einsum('bqgrk,bqkgd->bqgrd', p.astype(vs.dtype), vs)
        return o.reshape(b_, Q_BLOCK, ATTN_WIDTH)

    out = lax.map(block, jnp.arange(n_blk))
    return jnp.transpose(out, (1, 0, 2, 3)).reshape(b_, s_, ATTN_WIDTH)


def token_mixer(h, w_in, w_pool_grp, pool_scale, w_up_pool, w_up_attn, w_out, rope_a, rope_i):
    b_, s_, _ = h.shape
    proj = h @ w_in
    offs = [int(o) for o in np.cumsum(IN_SPLITS)[:-1]]
    u, q, k, v, qi, ki, wi, gl = jnp.split(proj, offs, axis=-1)
    q = partial_rope(q.reshape(b_, s_, N_HEADS, HEAD_DIM), *rope_a)
    k = partial_rope(k.reshape(b_, s_, N_KV_HEADS, HEAD_DIM), *rope_a)
    v = v.reshape(b_, s_, N_KV_HEADS, HEAD_DIM)
    qi = partial_rope(qi.reshape(b_, s_, IDX_HEADS, IDX_DIM), *rope_i)
    ki = partial_rope(ki, *rope_i)
    wi = wi * (IDX_HEADS ** -0.5 * IDX_DIM ** -0.5)
    y_pool = pool_mixer(u, w_pool_grp, pool_scale)
    y_attn = dsa_attention(q, k, v, qi, ki, wi)
    g_pool, g_attn = jnp.split(jax.nn.sigmoid(gl), N_BRANCHES, axis=-1)
    merged = g_pool * (y_pool @ w_up_pool) + g_attn * (y_attn @ w_up_attn)
    return merged @ w_out


def moe_ffn(h, w_router, b_router, w1, b1, w2, b2):
    b_, s_, d_ = h.shape
    xt = h.reshape(-1, d_)
    n_tok = xt.shape[0]
    logits = (xt @ w_router + b_router).astype(jnp.float32)
    top_val, top_idx = lax.top_k(logits, TOP_K)
    gates = jax.nn.softmax(top_val, axis=-1)
    n_slots = n_tok * TOP_K
    e_flat = top_idx.reshape(-1)
    order = jnp.argsort(e_flat)
    sorted_e = e_flat[order]
    sorted_tok = order // TOP_K
    counts = jnp.zeros((N_EXPERTS,), jnp.int32).at[e_flat].add(1)
    starts = jnp.cumsum(counts) - counts
    padded = (counts + MOE_BLOCK - 1) // MOE_BLOCK * MOE_BLOCK
    pad_end = jnp.cumsum(padded)
    pad_start = pad_end - padded
    dest_sorted = pad_start[sorted_e] + (jnp.arange(n_slots) - starts[sorted_e])
    n_blocks = -(-n_slots // MOE_BLOCK) + N_EXPERTS
    n_rows = n_blocks * MOE_BLOCK
    row_tok = jnp.zeros((n_rows,), jnp.int32).at[dest_sorted].set(sorted_tok.astype(jnp.int32))
    row_gate = jnp.zeros((n_rows,), jnp.float32).at[dest_sorted].set(gates.reshape(-1)[order])
    blk_start = jnp.arange(n_blocks) * MOE_BLOCK
    blk_expert = jnp.minimum(jnp.searchsorted(pad_end, blk_start, side='right'), N_EXPERTS - 1)
    xs = xt[row_tok].reshape(n_blocks, MOE_BLOCK, d_)

    def expert_block(args):
        xb, e = args
        hb = xb @ w1[e] + b1[e]
        glu = jnp.minimum(hb[:, :D_FF], SWIGLU_LIMIT)
        lin = jnp.clip(hb[:, D_FF:], -SWIGLU_LIMIT, SWIGLU_LIMIT)
        act = glu * jax.nn.sigmoid(SWIGLU_ALPHA * glu) * (lin + 1)
        return act @ w2[e] + b2[e]

    ys = lax.map(expert_block, (xs, blk_expert)).reshape(n_rows, d_)
    y = jax.ops.segment_sum(ys * row_gate[:, None].astype(ys.dtype), row_tok, num_segments=n_tok)
    return y.reshape(b_, s_, d_)


def setup_inputs(seed: int = 0) -> dict:
    key = jax.random.key(seed)
    ks = jax.random.split(key, 20)
    f32 = jnp.float32
    nrm = lambda k, shape, s: jax.random.normal(k, shape, f32) * s
    L = DEPTH
    return {
        'x': nrm(ks[0], (BATCH, SEQ, D_MODEL), 1.0),
        'c': nrm(ks[1], (BATCH, D_MODEL), 1.0),
        'w_ada': nrm(ks[2], (L, D_MODEL, N_MOD * D_MODEL), 0.5 * D_MODEL ** -0.5),
        'b_ada': nrm(ks[3], (L, N_MOD * D_MODEL), 0.01),
        'g_pre_mix': 1.0 + nrm(ks[4], (L, D_MODEL), 0.02),
        'g_post_mix': 1.0 + nrm(ks[5], (L, D_MODEL), 0.02),
        'w_in': nrm(ks[6], (L, D_MODEL, IN_WIDTH), D_MODEL ** -0.5),
        'w_pool_grp': nrm(ks[7], (L, POOL_GROUPS, POOL_GROUP_DIM, POOL_GROUP_DIM), POOL_GROUP_DIM ** -0.5),
        'pool_scale': 1.0 + nrm(ks[8], (L, POOL_WIDTH), 0.02),
        'w_up_pool': nrm(ks[9], (L, POOL_WIDTH, D_MODEL), POOL_WIDTH ** -0.5),
        'w_up_attn': nrm(ks[10], (L, ATTN_WIDTH, D_MODEL), ATTN_WIDTH ** -0.5),
        'w_out': nrm(ks[11], (L, D_MODEL, D_MODEL), D_MODEL ** -0.5),
        'g_pre_ffn': 1.0 + nrm(ks[12], (L, D_MODEL), 0.02),
        'g_post_ffn': 1.0 + nrm(ks[13], (L, D_MODEL), 0.02),
        'w_router': nrm(ks[14], (L, D_MODEL, N_EXPERTS), D_MODEL ** -0.5),
        'b_router': nrm(ks[15], (L, N_EXPERTS), 0.01),
        'w1': nrm(ks[16], (L, N_EXPERTS, D_MODEL, 2 * D_FF), D_MODEL ** -0.5),
        'b1': nrm(ks[17], (L, N_EXPERTS, 2 * D_FF), 0.01),
        'w2': nrm(ks[18], (L, N_EXPERTS, D_FF, D_MODEL), D_FF ** -0.5),
        'b2': nrm(ks[19], (L, N_EXPERTS, D_MODEL), 0.01),
    }


def reference(x, c, w_ada, b_ada, g_pre_mix, g_post_mix, w_in, w_pool_grp, pool_scale, w_up_pool, w_up_attn, w_out, g_pre_ffn, g_post_ffn, w_router, b_router, w1, b1, w2, b2):
    s_ = x.shape[1]
    rope_a = rope_tables(s_, ROT_DIM)
    rope_i = rope_tables(s_, IDX_ROT_DIM)
    c_act = jax.nn.silu(c)
    for layer in range(DEPTH):
        mod = (c_act @ w_ada[layer] + b_ada[layer])[:, None, :]
        sh1, sc1, gt1, sh2, sc2, gt2 = jnp.split(mod, N_MOD, axis=-1)
        h = rms_norm(x, g_pre_mix[layer]) * (1 + sc1) + sh1
        y = token_mixer(h, w_in[layer], w_pool_grp[layer], pool_scale[layer], w_up_pool[layer], w_up_attn[layer], w_out[layer], rope_a, rope_i)
        x = x + gt1 * rms_norm(y, g_post_mix[layer])
        h = rms_norm(x, g_pre_ffn[layer]) * (1 + sc2) + sh2
        y = moe_ffn(h, w_router[layer], b_router[layer], w1[layer], b1[layer], w2[layer], b2[layer])
        x = x + gt2 * rms_norm(y, g_post_ffn[layer])
    return x
```

```python
import numpy as np
from contextlib import ExitStack
import concourse.bass as bass
import concourse.mybir as mybir
from concourse.bass_utils import run_bass_kernel_spmd

F32 = mybir.dt.float32
BF16 = mybir.dt.bfloat16
AF = mybir.ActivationFunctionType
ALU = mybir.AluOpType
AX = mybir.AxisListType

D = 2048
T = 2048
NT = 16
NEXP = 32
EPS = 1e-6
ENGS = ("pe", "act", "dve", "pool", "sp")
CAP = 30000
SB_BASE = 16512
SB_END = 229376


class Op:
    __slots__ = ("eng", "fn", "reads", "writes", "dma_key", "ordinal", "waits", "signals",
                 "sigidx", "dma_cnt", "clock", "dma_clock", "alias")


def kname(k):
    return k[0] if isinstance(k, tuple) else k


class Sched:
    def __init__(self):
        self.ops = []
        self.per_eng = {e: [] for e in ENGS}
        self.dma_counts = {}

    def op(self, eng, fn, reads=(), writes=(), dma_key=None):
        o = Op()
        o.eng = eng
        o.fn = fn
        def _flat(xs):
            r = []
            for x_ in xs:
                if isinstance(x_, list):
                    r.extend(_flat(x_))
                else:
                    r.append(x_)
            return tuple(r)
        o.reads = _flat(reads)
        o.writes = _flat(writes)
        o.dma_key = dma_key
        o.signals = False
        o.sigidx = 0
        o.waits = []
        o.alias = None
        o.dma_cnt = 0
        o.ordinal = len(self.per_eng[eng])
        if dma_key is not None:
            self.dma_counts[dma_key] = self.dma_counts.get(dma_key, 0) + 1
            o.dma_cnt = self.dma_counts[dma_key]
        self.per_eng[eng].append(o)
        self.ops.append(o)
        return o

    def alias(self, new_name, old_names):
        o = Op()
        o.eng = None
        o.alias = (new_name, tuple(old_names))
        self.ops.append(o)

    def resolve(self):
        last_writer = {}
        readers = {}
        by_name = {}
        inherit = {}
        clock = {e: {} for e in ENGS}
        dclock = {e: {} for e in ENGS}

        def touch(k):
            if k not in readers:
                n = kname(k)
                readers[k] = list(inherit.get(n, ()))
                by_name.setdefault(n, []).append(k)

        for o in self.ops:
            if o.eng is None:
                new, olds = o.alias
                lst = inherit.setdefault(new, [])
                for on in olds:
                    lst.extend(inherit.get(on, ()))
                    for k in by_name.get(on, ()):
                        w = last_writer.get(k)
                        if w is not None:
                            lst.append(w)
                        lst.extend(readers.get(k, ()))
                best = {}
                for d in lst:
                    kk = ("d", d.dma_key) if d.dma_key is not None else ("e", d.eng)
                    val = d.dma_cnt if d.dma_key is not None else d.ordinal
                    if kk not in best or best[kk][0] < val:
                        best[kk] = (val, d)
                inherit[new] = [v[1] for v in best.values()]
                continue
            deps = []
            for r in o.reads:
                touch(r)
                w = last_writer.get(r)
                if w is not None:
                    deps.append(w)
                if kname(r) == "ps":
                    for k2 in by_name.get("ps", ()):
                        if k2[1] == r[1]:
                            deps.extend(o2 for o2 in readers[k2] if o2.eng != o.eng)
            for wk in o.writes:
                touch(wk)
                w = last_writer.get(wk)
                if w is not None:
                    deps.append(w)
                deps.extend(readers[wk])
            ck = clock[o.eng]
            dk = dclock[o.eng]
            need_e = {}
            need_d = {}
            for d in deps:
                if d is o:
                    continue
                if d.dma_key is not None:
                    if dk.get(d.dma_key, 0) >= d.dma_cnt:
                        continue
                    if need_d.get(d.dma_key, (0, None))[0] < d.dma_cnt:
                        need_d[d.dma_key] = (d.dma_cnt, d)
                else:
                    if d.eng == o.eng and o.eng == "pe" and o.dma_key is None:
                        continue
                    if ck.get(d.eng, -1) >= d.ordinal:
                        continue
                    if need_e.get(d.eng, (-1, None))[0] < d.ordinal:
                        need_e[d.eng] = (d.ordinal, d)
            waits = []
            for e, (ordn, d) in need_e.items():
                d.signals = True
                waits.append(("e", d))
                ck[e] = max(ck.get(e, -1), ordn)
                for e2, v in d.clock.items():
                    if e2 != o.eng and ck.get(e2, -1) < v:
                        ck[e2] = v
                for k2, v in d.dma_clock.items():
                    if dk.get(k2, 0) < v:
                        dk[k2] = v
            for k, (cnt, d) in need_d.items():
                waits.append(("d", d))
                dk[k] = max(dk.get(k, 0), cnt)
                for e2, v in d.clock.items():
                    if e2 != o.eng and ck.get(e2, -1) < v:
                        ck[e2] = v
                for k2, v in d.dma_clock.items():
                    if dk.get(k2, 0) < v:
                        dk[k2] = v
            o.waits = waits
            o.clock = dict(ck)
            o.dma_clock = dict(dk)
            for r in o.reads:
                readers[r].append(o)
            for wk in o.writes:
                last_writer[wk] = o
                readers[wk] = []
        self.nsig = {}
        for e in ENGS:
            n = 0
            for o in self.per_eng[e]:
                if o.dma_key is None and o.signals:
                    n += 1
                    o.sigidx = n
            self.nsig[e] = n

    def emit(self, nc, stack, final_waits=()):
        self.resolve()
        esems = {}
        for e in ENGS:
            nep = (self.nsig[e] + CAP - 1) // CAP
            esems[e] = [stack.enter_context(nc.semaphore(f"s_{e}_{i}")) for i in range(nep)]
        dsems = {}
        for i, k in enumerate(self.dma_counts):
            dsems[k] = stack.enter_context(nc.semaphore(f"d_{i}"))
        block = stack.enter_context(nc.Block())

        def run(e, engobj):
            for o in self.per_eng[e]:
                for kind, d in o.waits:
                    if kind == "e":
                        ep, val = (d.sigidx - 1) // CAP, (d.sigidx - 1) % CAP + 1
                        engobj.wait_ge(esems[d.eng][ep], val)
                    else:
                        engobj.wait_ge(dsems[d.dma_key], 16 * d.dma_cnt)
                ins = o.fn(engobj)
                if o.dma_key is not None:
                    ins.then_inc(dsems[o.dma_key], 16)
                elif o.signals:
                    ep = (o.sigidx - 1) // CAP
                    ins.then_inc(esems[e][ep], 1)
            if e == "sp":
                for k in final_waits:
                    engobj.wait_ge(dsems[k], 16 * self.dma_counts[k])

        @block.tensor
        def _(eng):
            run("pe", eng)

        @block.scalar
        def _(eng):
            run("act", eng)

        @block.vector
        def _(eng):
            run("dve", eng)

        @block.gpsimd
        def _(eng):
            run("pool", eng)

        @block.sync
        def _(eng):
            run("sp", eng)


class Arena:
    def __init__(self, nc, S):
        self.nc = nc
        self.S = S
        self.top = SB_BASE
        self.live = []
        self.freed = []
        self.cnt = 0

    def alloc(self, name, shape, dtype):
        esz = 4 if dtype == F32 else 2
        n = 1
        for s in shape[1:]:
            n *= s
        nbytes = (n * esz + 63) // 64 * 64
        start = self.top
        end = start + nbytes
        assert end <= SB_END, f"SBUF overflow allocating {name}: {end}"
        self.cnt += 1
        uname = f"{name}_{self.cnt}"
        h = self.nc.alloc_sbuf_tensor_at(uname, list(shape), dtype, offset=start)
        olds = [f[2] for f in self.freed if f[0] < end and start < f[1]]
        if olds:
            self.S.alias(uname, olds)
        self.live.append((start, end, uname))
        self.top = end
        return h, uname

    def mark(self):
        return (self.top, len(self.live))

    def release(self, m):
        top, n = m
        for rec in self.live[n:]:
            self.freed.append(rec)
        del self.live[n:]
        self.top = top


class _Stop(Exception):
    pass


def build(dbg=False, stop=None):
    nc = bass.Bass("TRN2", target_bir_lowering=False)
    S = Sched()

    def din(name, shape, dt=F32):
        return nc.dram_tensor(name, list(shape), dt, kind="ExternalInput").ap()

    x = din("x", [T, D])
    cT = din("cT", [128, 16])
    w_ada = din("w_ada", [D, 6 * D])
    b_ada = din("b_ada", [1, 6 * D])
    gvec = din("gvec", [4, D])
    w_in = din("w_in", [D, 9296])
    w_pool = din("w_pool", [4, 256, 256])
    pscale = din("pscale", [128, 8])
    w_up_pool = din("w_up_pool", [1024, D])
    w_up_attn = din("w_up_attn", [D, D])
    w_out = din("w_out", [D, D])
    w_router = din("w_router", [D, NEXP])
    b_router = din("b_router", [1, NEXP])
    w1 = din("w1", [NEXP, D, 2 * D] if stop is None else [1, 128, 128])
    b1T = din("b1T", [128, NEXP, 32])
    w2 = din("w2", [NEXP, D, D] if stop is None else [1, 128, 128])
    b2 = din("b2", [NEXP, D])
    identf = din("identf", [128, 128])
    ropeA = din("ropeA", [2, 128, T])
    ropeI = din("ropeI", [2, 128, T])
    perms = din("perms", [2, 128, 128])
    trineg = din("trineg", [128, 128])
    poolinv = din("poolinv", [128, 4, 16])

    kind_s = "ExternalOutput" if dbg else "Internal"
    out = nc.dram_tensor("out", [T, D], F32, kind="ExternalOutput").ap()
    modrow = nc.dram_tensor("modrow", [6, D], F32, kind=kind_s).ap()
    hT_d = nc.dram_tensor("hT_d", [128, 16, T], BF16, kind=kind_s).ap()
    ypT_d = nc.dram_tensor("ypT_d", [128, 8, T], BF16, kind=kind_s).ap()
    oT_d = nc.dram_tensor("oT_d", [128, 16, T], BF16, kind=kind_s).ap()
    x1_d = nc.dram_tensor("x1_d", [T, D], F32, kind=kind_s).ap()
    h2T_d = nc.dram_tensor("h2T_d", [128, 16, T], BF16, kind=kind_s).ap()
    G_d = nc.dram_tensor("G_d", [128, NT, NEXP], F32, kind=kind_s).ap()
    mk_d = nc.dram_tensor("mk_d", [T, T], BF16, kind=kind_s).ap() if dbg else None

    with ExitStack() as st:
        A = Arena(nc, S)
        PS = [st.enter_context(nc.psum_tensor(f"psb{i}", [128, 512], F32)) for i in range(8)]
        PSB = [p.bitcast(BF16) for p in PS]

        def psk(b, sub=None):
            return ("ps", b) if sub is None else ("ps", b, sub)

        def dma(eng, out_ap, in_ap, reads, writes, key):
            S.op(eng, lambda e: e.dma_start(out=out_ap, in_=in_ap), reads, writes, dma_key=key)

        def mm(out_ap, lhsT, rhs, start, stop, reads, writes):
            S.op("pe", lambda e: e.matmul(out_ap, lhsT=lhsT, rhs=rhs, start=start, stop=stop), reads, writes)

        def tr(out_ap, in_ap, ident, reads, writes):
            S.op("pe", lambda e: e.transpose(out=out_ap, in_=in_ap, identity=ident), reads, writes)

        def act(out_ap, in_ap, func, reads, writes, bias=None, scale=None, accum=None):
            kw = {}
            if bias is not None:
                kw["bias"] = bias
            if scale is not None:
                kw["scale"] = scale
            if accum is not None:
                kw["accum_out"] = accum
            S.op("act", lambda e: e.activation(out=out_ap, in_=in_ap, func=func, **kw), reads, writes)

        def ts(eng, out_ap, in0, s1, s2, op0, op1, reads, writes, accum=None):
            if op1 is None:
                S.op(eng, lambda e: e.tensor_scalar(out=out_ap, in0=in0, scalar1=s1, scalar2=None, op0=op0), reads, writes)
            elif accum is None:
                S.op(eng, lambda e: e.tensor_scalar(out=out_ap, in0=in0, scalar1=s1, scalar2=s2, op0=op0, op1=op1), reads, writes)
            else:
                S.op(eng, lambda e: e.tensor_scalar(out=out_ap, in0=in0, scalar1=s1, scalar2=s2, op0=op0, op1=op1, accum_out=accum), reads, writes)

        def tt(eng, out_ap, in0, in1, op, reads, writes):
            S.op(eng, lambda e: e.tensor_tensor(out=out_ap, in0=in0, in1=in1, op=op), reads, writes)

        def stt(out_ap, in0, scalar, in1, op0, op1, reads, writes):
            S.op("dve", lambda e: e.scalar_tensor_tensor(out=out_ap, in0=in0, scalar=scalar, in1=in1, op0=op0, op1=op1), reads, writes)

        def cp(eng, out_ap, in_ap, reads, writes):
            if eng == "act":
                S.op("act", lambda e: e.copy(out=out_ap, in_=in_ap), reads, writes)
            else:
                S.op(eng, lambda e: e.tensor_copy(out=out_ap, in_=in_ap), reads, writes)

        def bcast_row(dram_ap_row, n):
            return bass.AP(tensor=dram_ap_row.tensor, offset=dram_ap_row.offset, ap=[[0, 128], [1, n]])

        def rstd_from_ssq(ssq, nm):
            ts("dve", ssq, ssq, 1.0 / D, EPS, ALU.mult, ALU.add, [nm], [nm])
            act(ssq, ssq, AF.Sqrt, [nm], [nm])
            S.op("dve", lambda e: e.reciprocal(out=ssq, in_=ssq), [nm], [nm])

        idf, k_idf = A.alloc("identf", [128, 128], F32)
        idb, k_idb = A.alloc("identb", [128, 128], BF16)
        dma("sp", idf[:], identf[:, :], [], [k_idf], "c_idf")
        dma("pool", idb[:], identf[:, :], [], [k_idb], "c_idb")
        G_sb, k_G = A.alloc("G", [128, NT, NEXP], F32)

        w_in_v = w_in.rearrange("(kc p) n -> p kc n", p=128)

        def chk(name):
            if stop == name:
                raise _Stop()

        try:
            mA = A.mark()
            cact, k_cact = A.alloc("cact", [128, 16], F32)
            crep, k_crep = A.alloc("crep", [128, 16, 128], F32)
            modbc = []
            for j in range(6):
                modbc.append(A.alloc(f"modbc{j}", [128, D], F32))
            gbc, k_gbc = A.alloc("gbc", [128, D], F32)
            babc, k_babc = A.alloc("babc", [128, D], F32)
            wa = [A.alloc(f"wa{i}", [128, 16, 512], F32) for i in range(2)]
            dma("sp", cact[:], cT[:, :], [], [k_cact], "c_cact")
            act(cact[:], cact[:], AF.Silu, [k_cact], [k_cact])
            for kc in range(16):
                cp("dve", crep[:, kc, :], cact[:, kc:kc + 1].to_broadcast([128, 128]), [k_cact], [(k_crep, kc)])
            w_ada_v = w_ada.rearrange("(kc p) n -> p kc n", p=128)
            it = 0
            for j in range(6):
                dma("sp", babc[:], bcast_row(b_ada[0:1, j * D:(j + 1) * D], D), [], [k_babc], "c_babc")
                for cb in range(4):
                    slot = it % 2
                    wt, k_wt = wa[slot]
                    c0 = j * D + cb * 512
                    dma("sp", wt[:, :, :], w_ada_v[:, :, c0:c0 + 512], [], [k_wt], f"wa{slot}")
                    pb = it % 2
                    for kc in range(16):
                        mm(PS[pb][:, :], crep[:, kc, :], wt[:, kc, :], kc == 0, kc == 15,
                           [(k_crep, kc), k_wt], [psk(pb)])
                    tt("dve", modbc[j][0][:, cb * 512:(cb + 1) * 512], PS[pb][:, :], babc[:, cb * 512:(cb + 1) * 512],
                       ALU.add, [psk(pb), k_babc], [(modbc[j][1], cb)])
                    it += 1
            def allk(j):
                return [(modbc[j][1], cb) for cb in range(4)]

            def derive(jmod, grow, add_one, outrow):
                dma("sp", gbc[:], bcast_row(gvec[grow:grow + 1, :], D), [], [k_gbc], "c_gbc")
                if add_one:
                    stt(modbc[jmod][0][:, :], modbc[jmod][0][:, :], 1.0, gbc[:, :], ALU.add, ALU.mult,
                        allk(jmod) + [k_gbc], allk(jmod))
                else:
                    tt("dve", modbc[jmod][0][:, :], modbc[jmod][0][:, :], gbc[:, :], ALU.mult,
                       allk(jmod) + [k_gbc], allk(jmod))
                dma("sp", modrow[outrow:outrow + 1, :], modbc[jmod][0][0:1, :], allk(jmod), [("modrow", outrow)], f"modrow{outrow}")

            derive(1, 0, True, 0)
            dma("sp", modrow[1:2, :], modbc[0][0][0:1, :], allk(0), [("modrow", 1)], "modrow1")
            derive(2, 1, False, 2)
            derive(4, 2, True, 3)
            dma("sp", modrow[4:5, :], modbc[3][0][0:1, :], allk(3), [("modrow", 4)], "modrow4")
            derive(5, 3, False, 5)
            A.release(mA)
            chk("A")

            def load_bc(dst, k_dst, row, key):
                dma("sp", dst[:], bcast_row(modrow[row:row + 1, :], D), [("modrow", row)], [k_dst], key)

            mB = A.mark()
            hT, k_hT = A.alloc("hT", [128, 16, T], BF16)
            mB2 = A.mark()
            A1, k_A1 = A.alloc("A1", [128, D], F32)
            sh1, k_sh1 = A.alloc("sh1", [128, D], F32)
            load_bc(A1, k_A1, 0, "c_A1")
            load_bc(sh1, k_sh1, 1, "c_sh1")
            xt = [A.alloc(f"xt{i}", [128, D], F32) for i in range(2)]
            hb = [A.alloc(f"hb{i}", [128, D], BF16) for i in range(2)]
            junk, k_junk = A.alloc("junkB", [128, D], BF16)
            ssq = [A.alloc(f"ssq{i}", [128, 1], F32) for i in range(2)]

            def norm_mod_tile(src, k_src, dst, k_dst, Abc, k_Abc, shbc, k_shbc, ssq_t, k_ssq, junk_t, k_junk_t):
                act(junk_t[:], src[:], AF.Square, [k_src], [k_junk_t, k_ssq], accum=ssq_t[:])
                rstd_from_ssq(ssq_t[:], k_ssq)
                stt(src[:], src[:], ssq_t[:, 0:1], Abc[:], ALU.mult, ALU.mult, [k_src, k_ssq, k_Abc], [k_src])
                tt("pool", dst[:], src[:], shbc[:], ALU.add, [k_src, k_shbc], [k_dst])

            for t_ in range(NT):
                s_ = t_ % 2
                xt_, k_xt = xt[s_]
                hb_, k_hb = hb[s_]
                dma("sp", xt_[:], x[t_ * 128:(t_ + 1) * 128, :], [], [k_xt], f"xt{s_}")
                norm_mod_tile(xt_, k_xt, hb_, k_hb, A1, k_A1, sh1, k_sh1, ssq[s_][0], ssq[s_][1], junk, k_junk)
                b0 = 2 * s_
                for kc in range(16):
                    bb = b0 + kc // 8
                    tr(PSB[bb][:, (kc % 8) * 128:(kc % 8 + 1) * 128], hb_[:, kc * 128:(kc + 1) * 128], idb[:],
                       [k_hb, k_idb], [psk(bb)])
                for hh in range(2):
                    cp("act" if hh == 0 else "dve", hT[:, hh * 8:(hh + 1) * 8, t_ * 128:(t_ + 1) * 128],
                       PSB[b0 + hh][:, :].rearrange("p (a b) -> p a b", a=8), [psk(b0 + hh)], [(k_hT, t_)])
            A.release(mB2)
            for hh in range(4):
                dma("sp", hT_d[:, :, hh * 512:(hh + 1) * 512], hT[:, :, hh * 512:(hh + 1) * 512],
                    [(k_hT, t_) for t_ in range(hh * 4, hh * 4 + 4)], [("hT_d", hh)], f"hT_d{hh}")
            hT_keys = [(k_hT, t_) for t_ in range(NT)]
            chk("B")

            def hT_blk_keys(blk):
                return [(k_hT, t_) for t_ in range(blk * 4, blk * 4 + 4)]

            def load_w(dst_ap, src_ap, k_dst, key):
                dma("pool", dst_ap, src_ap, [], [k_dst], key)

            mC3 = A.mark()
            wb = [A.alloc(f"wbP{i}", [128, 16, 512], BF16) for i in range(2)]
            wgrp, k_wgrp = A.alloc("wgrp", [128, 4, 2, 256], BF16)
            psc, k_psc = A.alloc("psc", [128, 8], F32)
            pinv, k_pinv = A.alloc("pinv", [128, 4, 16], F32)
            ub = [A.alloc(f"ub{i}", [128, 16 + T], F32) for i in range(3)]
            mixT, k_mixT = A.alloc("mixT", [128, 8, T], BF16)
            t16, k_t16 = A.alloc("t16", [128, 16], F32)
            ypst = [A.alloc(f"ypst{i}", [128, 512], BF16) for i in range(2)]
            for g in range(4):
                load_w(wgrp[:, g, :, :], w_pool[g].rearrange("(cc p) d -> p cc d", p=128), (k_wgrp, g), f"c_wgrp{g}")
            dma("sp", psc[:], pscale[:, :], [], [k_psc], "c_psc")
            dma("sp", pinv[:, :, :], poolinv[:, :, :], [], [k_pinv], "c_pinv")
            for i in range(3):
                S.op("pool", lambda e, i=i: e.memset(ub[i][0][:, 0:16], 0.0), [], [(ub[i][1], "pad")])
            pbi = 0
            for grp in range(2):
                wt, k_wt = wb[grp % 2]
                load_w(wt[:, :, :], w_in_v[:, :, grp * 512:(grp + 1) * 512], k_wt, f"wbP{grp % 2}")
                for oc in range(4):
                    c = grp * 4 + oc
                    g = c // 2
                    u_, k_u = ub[0]
                    for blk in range(4):
                        pb = pbi % 2
                        pbi += 1
                        for kc in range(16):
                            mm(PS[pb][:, :], wt[:, kc, oc * 128:(oc + 1) * 128], hT[:, kc, blk * 512:(blk + 1) * 512],
                               kc == 0, kc == 15, [k_wt] + hT_blk_keys(blk), [psk(pb)])
                        cp("act", u_[:, 16 + blk * 512:16 + (blk + 1) * 512], PS[pb][:, :], [psk(pb)], [(k_u, blk)])
                    ukeys = [(k_u, b_) for b_ in range(4)] + [(k_u, "pad")]
                    cur, k_cur = u_, ukeys
                    dst_i = 1
                    d_ = 1
                    for step in range(g + 1):
                        nxt, k_nx = ub[dst_i]
                        tt("dve", nxt[:, 16:16 + T], cur[:, 16:16 + T], cur[:, 16 - d_:16 - d_ + T], ALU.add,
                           k_cur, [(k_nx, "all")])
                        cur, k_cur = nxt, [(k_nx, "all"), (k_nx, "pad")]
                        dst_i = 3 - dst_i
                        d_ *= 2
                    w_ = 2 ** (g + 1)
                    stt(mixT[:, c, :], cur[:, 16:16 + T], 1.0 / w_, u_[:, 16:16 + T], ALU.mult, ALU.subtract,
                        k_cur + ukeys, [(k_mixT, c)])
                    tt("dve", t16[:], cur[:, 16:32], pinv[:, g, :], ALU.mult, k_cur + [k_pinv], [k_t16])
                    tt("dve", mixT[:, c, 0:16], t16[:], u_[:, 16:32], ALU.subtract, [k_t16] + ukeys, [(k_mixT, c)])
            si = 0
            for g in range(4):
                for dd in range(2):
                    oc_ = g * 2 + dd
                    for blk in range(4):
                        pb = pbi % 2
                        pbi += 1
                        for cc in range(2):
                            mm(PS[pb][:, :], wgrp[:, g, cc, dd * 128:(dd + 1) * 128], mixT[:, g * 2 + cc, blk * 512:(blk + 1) * 512],
                               cc == 0, cc == 1, [(k_wgrp, g), (k_mixT, g * 2 + cc)], [psk(pb)])
                        ys, k_ys = ypst[si % 2]
                        si += 1
                        ts("dve", ys[:], PS[pb][:, :], psc[:, oc_:oc_ + 1], None, ALU.mult, None, [psk(pb), k_psc], [k_ys])
                        dma("sp", ypT_d[:, oc_, blk * 512:(blk + 1) * 512], ys[:], [k_ys], [("ypT_d", blk)], f"ypst{si % 2}")
            A.release(mC3)
            chk("C3")

            def rope(pb, rb, perm, k_perm, tab, k_tab, blk, raw_t, tmp1_t, tmp2_t, dst_ap, dst_keys, psl=slice(0, 128)):
                raw, k_raw = raw_t
                t1, k_t1 = tmp1_t
                t2, k_t2 = tmp2_t
                RM = 3
                if RM != 11:
                    cp("act", raw[:], PS[pb][:, :], [psk(pb)], [k_raw])
                if RM == 12:
                    cp("dve", t1[:], PS[pb][:, :], [psk(pb)], [k_t1])
                    return
                if RM == 11:
                    tt("dve", t1[:], PS[pb][:, :], tab[:, 0, blk * 512:(blk + 1) * 512], ALU.mult, [psk(pb), k_tab], [k_t1])
                    return
                if RM >= 2:
                    mm(PS[rb][:, :], perm, raw[:], True, True, [k_perm, k_raw], [psk(rb)])
                if RM == 0:
                    return
                tt("dve", t1[:], PS[pb][:, :], tab[:, 0, blk * 512:(blk + 1) * 512], ALU.mult, [psk(pb), k_tab], [k_t1])
                if RM == 10:
                    return
                if RM >= 3:
                    tt("dve", t2[:], PS[rb][:, :], tab[:, 1, blk * 512:(blk + 1) * 512], ALU.mult, [psk(rb), k_tab], [k_t2])
                    tt("pool", dst_ap, t1[psl, :], t2[psl, :], ALU.add, [k_t1, k_t2], dst_keys)
                else:
                    cp("pool", dst_ap, t1[psl, :], [k_t1], dst_keys)

            mC1 = A.mark()
            MOFF = []
            off = 0
            for j in range(NT):
                MOFF.append(off)
                off += T - 128 * j
            maskT, k_maskT = A.alloc("maskT", [128, off], BF16)
            mC1b = A.mark()
            qiT, k_qiT = A.alloc("qiT", [128, 8, T], BF16)
            kiT, k_kiT = A.alloc("kiT", [128, T], BF16)
            wi, k_wi = A.alloc("wi", [128, NT, 16], F32)
            wabs, k_wabs = A.alloc("wabs", [128, NT, 16], F32)
            wsgn, k_wsgn = A.alloc("wsgn", [128, NT, 16], F32)
            mC1c = A.mark()
            wb = [A.alloc(f"wbI{i}", [128, 16, 512], BF16) for i in range(2)]
            wkiA, k_wkiA = A.alloc("wkiA", [128, 16, 128], BF16)
            wkiB, k_wkiB = A.alloc("wkiB", [128, 16, 128], BF16)
            rtab, k_rtab = A.alloc("rtabI", [128, 2, T], F32)
            permI, k_permI = A.alloc("permI", [128, 128], BF16)
            raws = [A.alloc(f"rawI{i}", [128, 512], BF16) for i in range(2)]
            t1s = [A.alloc(f"t1I{i}", [128, 512], F32) for i in range(2)]
            t2s = [A.alloc(f"t2I{i}", [128, 512], F32) for i in range(2)]
            dma("sp", rtab[:, 0, :], ropeI[0], [], [(k_rtab, 0)], "c_rtabI0")
            dma("sp", rtab[:, 1, :], ropeI[1], [], [(k_rtab, 1)], "c_rtabI1")
            k_rt = [(k_rtab, 0), (k_rtab, 1)]
            dma("pool", permI[:], perms[1], [], [k_permI], "c_permI")
            load_w(wkiA[:, :, :], w_in_v[:, :, 5056:5184], k_wkiA, "c_wki0")
            load_w(wkiB[:, :, :], w_in_v[:, :, 5120:5248], k_wkiB, "c_wki1")
            ri = 0
            chk("C1a0")
            for grp in range(2):
                if grp == 1:
                    chk("C1a1")
                wt, k_wt = wb[grp % 2]
                load_w(wt[:, :, :], w_in_v[:, :, 4096 + grp * 512:4096 + (grp + 1) * 512], k_wt, f"wbI{grp % 2}")
                for oc in range(4):
                    c = grp * 4 + oc
                    for blk in range(4):
                        pb = ri % 2
                        for kc in range(16):
                            mm(PS[pb][:, :], wt[:, kc, oc * 128:(oc + 1) * 128], hT[:, kc, blk * 512:(blk + 1) * 512],
                               kc == 0, kc == 15, [k_wt] + hT_blk_keys(blk), [psk(pb)])
                        rope(pb, 2 + pb, permI[:], k_permI, rtab, k_rt[0:2], blk, raws[pb], t1s[pb], t2s[pb],
                             qiT[:, c, blk * 512:(blk + 1) * 512], [(k_qiT, c, blk)])
                        ri += 1
            for blk in range(4):
                for (wk__, k_wk__, psl_, hf_) in ((wkiA, k_wkiA, slice(64, 128), 1), (wkiB, k_wkiB, slice(0, 64), 0)):
                    pb = ri % 2
                    for kc in range(16):
                        mm(PS[pb][:, :], wk__[:, kc, :], hT[:, kc, blk * 512:(blk + 1) * 512], kc == 0, kc == 15,
                           [k_wk__] + hT_blk_keys(blk), [psk(pb)])
                    rope(pb, 2 + pb, permI[:], k_permI, rtab, k_rt, blk, raws[pb], t1s[pb], t2s[pb],
                         kiT[psl_, blk * 512:(blk + 1) * 512], [(k_kiT, blk, hf_)], psl=psl_)
                    ri += 1
            chk("C1a2")
            for t_ in range(NT):
                pb = 4 + t_ % 2
                for kc in range(16):
                    mm(PS[pb][:, 0:16], hT[:, kc, t_ * 128:(t_ + 1) * 128], wkiB[:, kc, 64:80], kc == 0, kc == 15,
                       [k_wkiB, (k_hT, t_)], [psk(pb)])
                ts("dve", wi[:, t_, :], PS[pb][:, 0:16], 1.0 / 32.0, None, ALU.mult, None, [psk(pb)], [(k_wi, t_)])
            wi_keys = [(k_wi, t_) for t_ in range(NT)]
            chk("C1a3")
            act(wsgn[:, :, :], wi[:, :, :], AF.Sign, wi_keys, [k_wsgn])
            tt("dve", wabs[:, :, :], wi[:, :, :], wsgn[:, :, :], ALU.mult, wi_keys + [k_wsgn], [k_wabs])
            A.release(mC1c)
            chk("C1a")
            sc = [A.alloc(f"sc{i}", [128, T], F32) for i in range(2)]
            rr = [A.alloc(f"rr{i}", [128, 512], F32) for i in range(4)]
            mk = [A.alloc(f"mk{i}", [128, T], BF16) for i in range(2)]
            jnk, k_jnk = A.alloc("jnkI", [128, T], BF16)
            tneg, k_tneg = A.alloc("tneg", [128, 128], F32)
            sm = [A.alloc(f"sm{i}", [128, 8], F32) for i in range(2)]
            dma("sp", tneg[:], trineg[:, :], [], [k_tneg], "c_tneg")
            NIT = 22
            rri = 0
            qi_keys_c = lambda c, i: [(k_qiT, c, i // 4)]
            for i in range(NT):
                L = 128 * (i + 1)
                nsb = (L + 511) // 512
                s0, k_s0 = sc[0]
                s1, k_s1 = sc[1]
                for h in range(16):
                    c, half = h // 2, h % 2
                    base = 64 * half
                    acc, k_acc = (s0, k_s0) if half == 0 else (s1, k_s1)
                    for sb in range(nsb):
                        n = min(512, L - 512 * sb)
                        pb = rri % 4
                        r_, k_r = rr[rri % 4]
                        rri += 1
                        mm(PS[pb][:, 0:n], qiT[base:base + 64, c, i * 128:(i + 1) * 128], kiT[base:base + 64, sb * 512:sb * 512 + n],
                           True, True, [(k_qiT, c, i // 4), (k_kiT, sb, half)], [psk(pb)])
                        act(r_[:, 0:n], PS[pb][:, 0:n], AF.Relu, [psk(pb), k_wabs], [k_r], scale=wabs[:, i, h:h + 1])
                        if h < 2:
                            ts("dve", acc[:, sb * 512:sb * 512 + n], r_[:, 0:n], wsgn[:, i, h:h + 1], None, ALU.mult, None,
                               [k_r, k_wsgn], [(k_acc, sb)])
                        else:
                            stt(acc[:, sb * 512:sb * 512 + n], r_[:, 0:n], wsgn[:, i, h:h + 1], acc[:, sb * 512:sb * 512 + n],
                                ALU.mult, ALU.add, [k_r, k_wsgn, (k_acc, sb)], [(k_acc, sb)])
                sk0 = [(k_s0, sb) for sb in range(nsb)]
                sk1 = [(k_s1, sb) for sb in range(nsb)]
                tt("pool", s0[:, 0:L], s0[:, 0:L], s1[:, 0:L], ALU.add, sk0 + sk1, sk0)
                sm_, k_sm = sm[i % 2]
                mk_, k_mk = mk[i % 2]
                if i >= 2:
                    S.op("dve", lambda e, s0=s0, sm_=sm_, L=L: e.tensor_reduce(out=sm_[:, 5:6], in_=s0[:, 0:L], axis=AX.X, op=ALU.max),
                         sk0, [(k_sm, 5)])
                    S.op("dve", lambda e, s0=s0, sm_=sm_, L=L: e.tensor_reduce(out=sm_[:, 0:1], in_=s0[:, 0:L], axis=AX.X, op=ALU.min),
                         sk0, [(k_sm, 0)])
                    tt("dve", sm_[:, 1:2], sm_[:, 5:6], sm_[:, 0:1], ALU.subtract, [(k_sm, 5), (k_sm, 0)], [(k_sm, 1)])
                tt("dve", s0[:, L - 128:L], s0[:, L - 128:L], tneg[:], ALU.add, sk0 + [k_tneg], sk0)
                if i >= 2:
                    for it_ in range(NIT):
                        f = 2.0 ** (-(it_ + 1))
                        stt(sm_[:, 2:3], sm_[:, 1:2], f, sm_[:, 0:1], ALU.mult, ALU.add, [(k_sm, 1), (k_sm, 0)], [(k_sm, 2)])
                        ts("dve", jnk[:, 0:L], s0[:, 0:L], sm_[:, 2:3], 0.0, ALU.is_ge, ALU.add, sk0 + [(k_sm, 2)],
                           [k_jnk, (k_sm, 3)], accum=sm_[:, 3:4])
                        ts("dve", sm_[:, 4:5], sm_[:, 3:4], 255.5, f, ALU.is_ge, ALU.mult, [(k_sm, 3)], [(k_sm, 4)])
                        stt(sm_[:, 0:1], sm_[:, 4:5], sm_[:, 1:2], sm_[:, 0:1], ALU.mult, ALU.add,
                            [(k_sm, 4), (k_sm, 1), (k_sm, 0)], [(k_sm, 0)])
                    ts("dve", mk_[:, 0:L], s0[:, 0:L], sm_[:, 0:1], None, ALU.is_ge, None, sk0 + [(k_sm, 0)], [k_mk])
                else:
                    ts("dve", mk_[:, 0:L], s0[:, 0:L], -1.0e3, None, ALU.is_ge, None, sk0, [k_mk])
                if dbg:
                    dma("sp", mk_d[i * 128:(i + 1) * 128, 0:L], mk_[:, 0:L], [k_mk], [("mk_d", i)], f"mkd{i % 2}")
                for j in range(i + 1):
                    bb = 4 + (j // 8) + 2 * (i % 2)
                    tr(PSB[bb][:, (j % 8) * 128:(j % 8 + 1) * 128], mk_[:, j * 128:(j + 1) * 128], idb[:], [k_mk, k_idb], [psk(bb)])
                for j in range(i + 1):
                    bb = 4 + (j // 8) + 2 * (i % 2)
                    o_ = MOFF[j] + (i - j) * 128
                    cp("act" if j % 2 == 0 else "dve", maskT[:, o_:o_ + 128], PSB[bb][:, (j % 8) * 128:(j % 8 + 1) * 128],
                       [psk(bb)], [(k_maskT, j, i)])
            A.release(mC1b)
            chk("C1")

            mC2 = A.mark()
            wb = [A.alloc(f"wbA{i}", [128, 16, 512], BF16) for i in range(2)]
            wkv = [A.alloc(f"wkv{i}", [128, 16, 256], BF16) for i in range(1)]
            rtabA, k_rtabA = A.alloc("rtabA", [128, 2, T], F32)
            permA, k_permA = A.alloc("permA", [128, 128], BF16)
            raws = [A.alloc(f"rawA{i}", [128, 512], BF16) for i in range(2)]
            t1s = [A.alloc(f"t1A{i}", [128, 512], F32) for i in range(2)]
            t2s = [A.alloc(f"t2A{i}", [128, 512], F32) for i in range(2)]
            qT = [A.alloc(f"qT{i}", [128, 4, T], BF16) for i in range(1)]
            kT_, k_kT = A.alloc("kT", [128, T], BF16)
            vg, k_vg = A.alloc("vg", [128, NT, 130], BF16)
            ex = [A.alloc(f"ex{i}", [128, 512], BF16) for i in range(3)]
            pp = [A.alloc(f"pp{i}", [128, 512], BF16) for i in range(3)]
            rden = [A.alloc(f"rden{i}", [128, 1], F32) for i in range(2)]
            otm = [A.alloc(f"otm{i}", [128, 128], BF16) for i in range(2)]
            ost = [A.alloc(f"ost{i}", [128, 512], BF16) for i in range(2)]
            dma("sp", rtabA[:, 0, :], ropeA[0], [], [(k_rtabA, 0)], "c_rtabA0")
            dma("sp", rtabA[:, 1, :], ropeA[1], [], [(k_rtabA, 1)], "c_rtabA1")
            k_rtA = [(k_rtabA, 0), (k_rtabA, 1)]
            dma("pool", permA[:], perms[0], [], [k_permA], "c_permA")
            SCL = 128.0 ** -0.5
            ri = 0
            exi = 0
            osti = 0
            for g in range(4):
                wt, k_wt = wb[g % 2]
                wk_, k_wk = wkv[0]
                load_w(wt[:, :, :], w_in_v[:, :, 1024 + g * 512:1024 + (g + 1) * 512], k_wt, f"wbA{g % 2}")
                load_w(wk_[:, :, 0:128], w_in_v[:, :, 3072 + g * 128:3072 + (g + 1) * 128], (k_wk, 0), "wkv_a")
                load_w(wk_[:, :, 128:256], w_in_v[:, :, 3584 + g * 128:3584 + (g + 1) * 128], (k_wk, 1), "wkv_b")
                q_, k_q = qT[0]
                for r in range(4):
                    for blk in range(4):
                        pb = ri % 2
                        for kc in range(16):
                            mm(PS[pb][:, :], wt[:, kc, r * 128:(r + 1) * 128], hT[:, kc, blk * 512:(blk + 1) * 512],
                               kc == 0, kc == 15, [k_wt] + hT_blk_keys(blk), [psk(pb)])
                        rope(pb, 2 + pb, permA[:], k_permA, rtabA, k_rtA, blk, raws[pb], t1s[pb], t2s[pb],
                             q_[:, r, blk * 512:(blk + 1) * 512], [(k_q, r, blk)])
                        ri += 1
                for blk in range(4):
                    pb = ri % 2
                    for kc in range(16):
                        mm(PS[pb][:, :], wk_[:, kc, 0:128], hT[:, kc, blk * 512:(blk + 1) * 512], kc == 0, kc == 15,
                           [(k_wk, 0)] + hT_blk_keys(blk), [psk(pb)])
                    rope(pb, 2 + pb, permA[:], k_permA, rtabA, k_rtA, blk, raws[pb], t1s[pb], t2s[pb],
                         kT_[:, blk * 512:(blk + 1) * 512], [(k_kT, blk)])
                    ri += 1
                S.op("pool", lambda e: e.memset(vg[:, :, 128:130], 1.0), [], [(k_vg, "one")])
                for t_ in range(NT):
                    pb = ri % 2
                    ri += 1
                    for kc in range(16):
                        mm(PS[pb][:, 0:128], hT[:, kc, t_ * 128:(t_ + 1) * 128], wk_[:, kc, 128:256], kc == 0, kc == 15,
                           [(k_wk, 1), (k_hT, t_)], [psk(pb)])
                    cp("act", vg[:, t_, 0:128], PS[pb][:, 0:128], [psk(pb)], [(k_vg, t_)])
                for r in range(4):
                    h = 4 * g + r
                    for qb in range(4):
                        nj = 4 * qb + 4
                        for j in range(nj):
                            t_lo = max(qb * 512, j * 128)
                            n = qb * 512 + 512 - t_lo
                            pb = exi % 2
                            e_, k_e = ex[exi % 3]
                            p_, k_p = pp[exi % 3]
                            exi += 1
                            mm(PS[pb][:, 0:n], kT_[:, j * 128:(j + 1) * 128], q_[:, r, t_lo:t_lo + n], True, True,
                               [(k_kT, j // 4), (k_q, r, qb)], [psk(pb)])
                            act(e_[:, 0:n], PS[pb][:, 0:n], AF.Exp, [psk(pb)], [k_e], scale=SCL)
                            mo = MOFF[j] + (t_lo - 128 * j)
                            mkeys = [(k_maskT, j, i_) for i_ in range(t_lo // 128, t_lo // 128 + n // 128)]
                            tt("dve" if exi % 2 == 0 else "pool", p_[:, 0:n], e_[:, 0:n], maskT[:, mo:mo + n], ALU.mult,
                               [k_e] + mkeys, [k_p])
                            for ts_ in range((t_lo - qb * 512) // 128, 4):
                                col = (qb * 512 + ts_ * 128) - t_lo
                                ab = 4 + ts_
                                ac = 0
                                jlast = 4 * qb + ts_
                                mm(PS[ab][:, ac:ac + 129], p_[:, col:col + 128], vg[:, j, 0:129], j == 0, j == jlast,
                                   [k_p, (k_vg, j), (k_vg, "one")], [psk(ab)])
                        os_, k_os = ost[osti % 2]
                        osti += 1
                        for ts_ in range(4):
                            ab = 4 + ts_
                            ac = 0
                            rd, k_rd = rden[ts_ % 2]
                            om, k_om = otm[ts_ % 2]
                            S.op("dve", lambda e, rd=rd, ab=ab, ac=ac: e.reciprocal(out=rd[:], in_=PS[ab][:, ac + 128:ac + 129]),
                                 [psk(ab)], [k_rd])
                            ts("dve", om[:], PS[ab][:, ac:ac + 128], rd[:, 0:1], None, ALU.mult, None,
                               [psk(ab), k_rd], [k_om])
                            tb = 2 + ts_ % 2
                            tr(PSB[tb][:, 0:128], om[:], idb[:], [k_om, k_idb], [psk(tb)])
                            cp("act", os_[:, ts_ * 128:(ts_ + 1) * 128], PSB[tb][:, 0:128], [psk(tb)], [(k_os, ts_)])
                        dma("sp", oT_d[:, h, qb * 512:(qb + 1) * 512], os_[:], [(k_os, ts_) for ts_ in range(4)],
                            [("oT_d", qb)], f"ost{osti % 2}")
            A.release(mC2)
            A.release(mC1)
            A.release(mB)
            chk("C2")

            mC4 = A.mark()
            wup = [A.alloc(f"wup{i}", [128, 56, 128], BF16) for i in range(2)]
            hTb, k_hTb = A.alloc("hTb", [128, 16, 512], BF16)
            ypb, k_ypb = A.alloc("ypb", [128, 8, 512], BF16)
            oTb, k_oTb = A.alloc("oTb", [128, 16, 512], BF16)
            mrg, k_mrg = A.alloc("mrg", [128, 16, 512], BF16)
            wo = [A.alloc(f"wo{i}", [128, 16, 512], BF16) for i in range(2)]
            sg = [A.alloc(f"sg{i}", [128, 512], F32) for i in range(4)]
            xt1, k_xt1 = A.alloc("xt1", [128, D], F32)
            h2t, k_h2t = A.alloc("h2t", [128, D], F32)
            gt1g, k_gt1g = A.alloc("gt1g", [128, D], F32)
            A2, k_A2 = A.alloc("A2", [128, D], F32)
            sh2, k_sh2 = A.alloc("sh2", [128, D], F32)
            h2Tf, k_h2Tf = A.alloc("h2Tf", [128, 16, 128], F32)
            h2Tb, k_h2Tb = A.alloc("h2Tb", [128, 16, 128], BF16)
            junk4, k_junk4 = A.alloc("junk4", [128, D], BF16)
            ssq4 = [A.alloc(f"ssq4{i}", [128, 1], F32) for i in range(2)]
            ssp, k_ssp = A.alloc("ssp", [128, 4], F32)
            wr, k_wr = A.alloc("wr", [128, 16, NEXP], F32)
            brbc, k_brbc = A.alloc("brbc", [128, NEXP], F32)
            lg, k_lg = A.alloc("lg", [128, NEXP], F32)
            m8, k_m8 = A.alloc("m8", [128, 8], F32)
            sel, k_sel = A.alloc("sel", [128, NEXP], F32)
            ee, k_ee = A.alloc("ee", [128, NEXP], F32)
            ssm, k_ssm = A.alloc("ssm", [128, 2], F32)
            load_bc(gt1g, k_gt1g, 2, "c_gt1g")
            load_bc(A2, k_A2, 3, "c_A2")
            load_bc(sh2, k_sh2, 4, "c_sh2")
            dma("sp", wr[:, :, :], w_router.rearrange("(kc p) n -> p kc n", p=128), [], [k_wr], "c_wr")
            dma("sp", brbc[:], bcast_row(b_router[0:1, :], NEXP), [], [k_brbc], "c_brbc")
            wupP_v = w_up_pool.rearrange("(kc p) n -> p kc n", p=128)
            wupA_v = w_up_attn.rearrange("(kc p) n -> p kc n", p=128)
            wout_v = w_out.rearrange("(kc p) n -> p kc n", p=128)
            ui = 0
            woi = 0
            for blk in range(4):
                tsl = slice(blk * 512, (blk + 1) * 512)
                dma("sp", hTb[:, :, :], hT_d[:, :, tsl], [("hT_d", blk)], [k_hTb], "c_hTb")
                dma("sp", ypb[:, :, :], ypT_d[:, :, tsl], [("ypT_d", blk)], [k_ypb], "c_ypb")
                dma("sp", oTb[:, :, :], oT_d[:, :, tsl], [("oT_d", blk)], [k_oTb], "c_oTb")
                for dc in range(16):
                    wu, k_wu = wup[ui % 2]
                    key = f"wup{ui % 2}"
                    ui += 1
                    dsl = slice(dc * 128, (dc + 1) * 128)
                    load_w(wu[:, 0:8, :], wupP_v[:, :, dsl], (k_wu, 0), key + "a")
                    load_w(wu[:, 8:24, :], wupA_v[:, :, dsl], (k_wu, 1), key + "b")
                    load_w(wu[:, 24:40, :], w_in_v[:, :, 5200 + dc * 128:5200 + (dc + 1) * 128], (k_wu, 2), key + "c")
                    load_w(wu[:, 40:56, :], w_in_v[:, :, 7248 + dc * 128:7248 + (dc + 1) * 128], (k_wu, 3), key + "d")
                    for kc in range(8):
                        mm(PS[0][:, :], wu[:, kc, :], ypb[:, kc, :], kc == 0, kc == 7, [(k_wu, 0), k_ypb], [psk(0)])
                    for kc in range(16):
                        mm(PS[1][:, :], wu[:, 8 + kc, :], oTb[:, kc, :], kc == 0, kc == 15, [(k_wu, 1), k_oTb], [psk(1)])
                    for kc in range(16):
                        mm(PS[2][:, :], wu[:, 24 + kc, :], hTb[:, kc, :], kc == 0, kc == 15, [(k_wu, 2), k_hTb], [psk(2)])
                    for kc in range(16):
                        mm(PS[3][:, :], wu[:, 40 + kc, :], hTb[:, kc, :], kc == 0, kc == 15, [(k_wu, 3), k_hTb], [psk(3)])
                    act(sg[0][0][:], PS[2][:, :], AF.Sigmoid, [psk(2)], [sg[0][1]])
                    act(sg[1][0][:], PS[3][:, :], AF.Sigmoid, [psk(3)], [sg[1][1]])
                    tt("dve", sg[2][0][:], PS[0][:, :], sg[0][0][:], ALU.mult, [psk(0), sg[0][1]], [sg[2][1]])
                    tt("dve", sg[3][0][:], PS[1][:, :], sg[1][0][:], ALU.mult, [psk(1), sg[1][1]], [sg[3][1]])
                    tt("pool", mrg[:, dc, :], sg[2][0][:], sg[3][0][:], ALU.add, [sg[2][1], sg[3][1]], [(k_mrg, dc)])
                mrg_keys = [(k_mrg, dc) for dc in range(16)]
                for ts_ in range(4):
                    t_ = blk * 4 + ts_
                    for db in range(4):
                        wo_, k_wo = wo[woi % 2]
                        key = f"wo{woi % 2}"
                        woi += 1
                        load_w(wo_[:, :, :], wout_v[:, :, db * 512:(db + 1) * 512], k_wo, key)
                        for kc in range(16):
                            mm(PS[4 + db][:, :], mrg[:, kc, ts_ * 128:(ts_ + 1) * 128], wo_[:, kc, :], kc == 0, kc == 15,
                               [k_wo, (k_mrg, kc)], [psk(4 + db)])
                    sq_, k_sq = ssq4[0]
                    ypk = [psk(4 + db) for db in range(4)]
                    for db in range(4):
                        act(junk4[:, db * 512:(db + 1) * 512], PS[4 + db][:, :], AF.Square, [psk(4 + db)],
                            [(k_junk4, db), (k_ssp, db)], accum=ssp[:, db:db + 1])
                    S.op("dve", lambda e, sq_=sq_: e.tensor_reduce(out=sq_[:], in_=ssp[:, 0:4], axis=AX.X, op=ALU.add),
                         [(k_ssp, db) for db in range(4)], [k_sq])
                    rstd_from_ssq(sq_[:], k_sq)
                    dma("sp", xt1[:], x[t_ * 128:(t_ + 1) * 128, :], [], [k_xt1], "c_xt1")
                    for db in range(4):
                        dsl = slice(db * 512, (db + 1) * 512)
                        stt(h2t[:, dsl], PS[4 + db][:, :], sq_[:, 0:1], gt1g[:, dsl], ALU.mult, ALU.mult,
                            [psk(4 + db), k_sq, k_gt1g], [(k_h2t, db)])
                    h2k = [(k_h2t, db) for db in range(4)]
                    tt("pool", xt1[:], xt1[:], h2t[:], ALU.add, [k_xt1] + h2k, [k_xt1])
                    dma("sp", x1_d[t_ * 128:(t_ + 1) * 128, :], xt1[:], [k_xt1], [("x1_d", t_)], "c_x1st")
                    sq2, k_sq2 = ssq4[1]
                    act(junk4[:], xt1[:], AF.Square, [k_xt1], [(k_junk4, db) for db in range(4)] + [k_sq2], accum=sq2[:])
                    rstd_from_ssq(sq2[:], k_sq2)
                    stt(h2t[:], xt1[:], sq2[:, 0:1], A2[:], ALU.mult, ALU.mult, [k_xt1, k_sq2, k_A2], h2k)
                    tt("pool", h2t[:], h2t[:], sh2[:], ALU.add, h2k + [k_sh2], h2k)
                    for kc in range(16):
                        bb = kc // 4
                        tr(PS[bb][:, (kc % 4) * 128:(kc % 4 + 1) * 128], h2t[:, kc * 128:(kc + 1) * 128], idf[:],
                           h2k + [k_idf], [psk(bb)])
                    for bb in range(4):
                        cp("act", h2Tf[:, bb * 4:(bb + 1) * 4, :], PS[bb][:, :].rearrange("p (a b) -> p a b", a=4),
                           [psk(bb)], [(k_h2Tf, bb)])
                        cp("dve", h2Tb[:, bb * 4:(bb + 1) * 4, :], PS[bb][:, :].rearrange("p (a b) -> p a b", a=4),
                           [psk(bb)], [(k_h2Tb, bb)])
                    dma("sp", h2T_d[:, :, t_ * 128:(t_ + 1) * 128], h2Tb[:, :, :], [(k_h2Tb, bb) for bb in range(4)],
                        [("h2T_d", t_)], "c_h2Tst")
                    for kc in range(16):
                        mm(PS[0][:, 0:NEXP], h2Tf[:, kc, :], wr[:, kc, :], kc == 0, kc == 15,
                           [(k_h2Tf, kc // 4), k_wr], [psk(0)])
                    tt("dve", lg[:], PS[0][:, 0:NEXP], brbc[:], ALU.add, [psk(0), k_brbc], [k_lg])
                    S.op("dve", lambda e: e.max(out=m8[:], in_=lg[:]), [k_lg], [k_m8])
                    ts("dve", sel[:], lg[:], m8[:, 3:4], None, ALU.is_ge, None, [k_lg, k_m8], [k_sel])
                    ts("dve", ssm[:, 0:1], m8[:, 0:1], -1.0, None, ALU.mult, None, [k_m8], [(k_ssm, 0)])
                    act(ee[:], lg[:], AF.Exp, [k_lg, (k_ssm, 0)], [k_ee], bias=ssm[:, 0:1])
                    tt("dve", ee[:], ee[:], sel[:], ALU.mult, [k_ee, k_sel], [k_ee])
                    S.op("dve", lambda e: e.tensor_reduce(out=ssm[:, 1:2], in_=ee[:], axis=AX.X, op=ALU.add), [k_ee], [(k_ssm, 1)])
                    S.op("dve", lambda e: e.reciprocal(out=ssm[:, 1:2], in_=ssm[:, 1:2]), [(k_ssm, 1)], [(k_ssm, 1)])
                    ts("dve", G_sb[:, t_, :], ee[:], ssm[:, 1:2], None, ALU.mult, None, [k_ee, (k_ssm, 1)], [(k_G, t_)])
            if dbg:
                dma("sp", G_d[:, :, :], G_sb[:, :, :], [(k_G, t_) for t_ in range(NT)], ["G_d"], "c_Gd")
            A.release(mC4)
            chk("C4")

            mD = A.mark()
            h2h, k_h2h = A.alloc("h2h", [128, 16, 1024], BF16)
            yacc, k_yacc = A.alloc("yacc", [128, 8, D], F32)
            actT, k_actT = A.alloc("actT", [128, 8, 1024], BF16)
            w1g = [A.alloc(f"w1g{i}", [128, 16, 256], BF16) for i in range(2)]
            w1l = [A.alloc(f"w1l{i}", [128, 16, 256], BF16) for i in range(2)]
            w2b = [A.alloc(f"w2b{i}", [128, 8, 512], BF16) for i in range(2)]
            b1s, k_b1s = A.alloc("b1s", [128, NEXP, 32], F32)
            b2s, k_b2s = A.alloc("b2s", [NEXP, D], F32)
            GT, k_GT = A.alloc("GT", [NEXP, 128], F32)
            epi_t, k_epi = A.alloc("epi", [128, 8, 512], F32)
            eg = [(epi_t[:, i, :], (k_epi, i)) for i in range(0, 2)]
            es_ = [(epi_t[:, 2 + i, :], (k_epi, 2 + i)) for i in range(0, 2)]
            el = [(epi_t[:, 4 + i, :], (k_epi, 4 + i)) for i in range(0, 2)]
            ea = [(epi_t[:, 6 + i, :], (k_epi, 6 + i)) for i in range(0, 2)]
            gt2g, k_gt2g = A.alloc("gt2g", [128, D], F32)
            ssqD, k_ssqD = A.alloc("ssqD", [128, 1], F32)
            dma("sp", b1s[:, :, :], b1T[:, :, :], [], [k_b1s], "c_b1s")
            dma("sp", b2s[:, :], b2[:, :], [], [k_b2s], "c_b2s")
            load_bc(gt2g, k_gt2g, 5, "c_gt2g")
            w1_v = w1.rearrange("e (kc p) n -> e p kc n", p=128)
            w2_v = w2.rearrange("e (kc p) n -> e p kc n", p=128)
            G_keys = [(k_G, t_) for t_ in range(NT)]
            wi1 = 0
            wi2 = 0
            epi = 0
            for half in range(2):
                for q4 in range(2):
                    b_ = half * 2 + q4
                    dma("sp", h2h[:, :, q4 * 512:(q4 + 1) * 512], h2T_d[:, :, b_ * 512:(b_ + 1) * 512],
                        [("h2T_d", t_) for t_ in range(b_ * 4, b_ * 4 + 4)], [(k_h2h, q4)], f"c_h2h{q4}")
                for tl in range(8):
                    t_ = half * 8 + tl
                    tr(PS[6][0:NEXP, 0:128], G_sb[:, t_, :], idf[:], [(k_G, t_), k_idf], [psk(6)])
                    cp("act", GT[:, :], PS[6][0:NEXP, 0:128], [psk(6)], [k_GT])
                    for db in range(4):
                        pb = 4 + db % 2
                        mm(PS[pb][:, :], GT[:, :], b2s[:, db * 512:(db + 1) * 512], True, True, [k_GT, k_b2s], [psk(pb)])
                        cp("dve", yacc[:, tl, db * 512:(db + 1) * 512], PS[pb][:, :], [psk(pb)], [(k_yacc, tl, db)])
                for e_ in range(NEXP):
                    for fh in range(2):
                        for fg in range(4):
                            fc0 = fh * 8 + fg * 2
                            wg_, k_wg = w1g[wi1 % 2]
                            wl_, k_wl = w1l[wi1 % 2]
                            kg, kl = f"w1g{wi1 % 2}", f"w1l{wi1 % 2}"
                            wi1 += 1
                            load_w(wg_[:, :, :], w1_v[e_][:, :, fc0 * 128:fc0 * 128 + 256], k_wg, kg)
                            load_w(wl_[:, :, :], w1_v[e_][:, :, D + fc0 * 128:D + fc0 * 128 + 256], k_wl, kl)
                            for fci in range(2):
                                fc = fc0 + fci
                                fl = fg * 2 + fci
                                for blk in range(2):
                                    pg = (epi % 2) * 2
                                    pl = pg + 1
                                    s_ = epi % 2
                                    epi += 1
                                    for kc in range(16):
                                        mm(PS[pg][:, :], wg_[:, kc, fci * 128:(fci + 1) * 128], h2h[:, kc, blk * 512:(blk + 1) * 512],
                                           kc == 0, kc == 15, [k_wg, (k_h2h, blk)], [psk(pg)])
                                    for kc in range(16):
                                        mm(PS[pl][:, :], wl_[:, kc, fci * 128:(fci + 1) * 128], h2h[:, kc, blk * 512:(blk + 1) * 512],
                                           kc == 0, kc == 15, [k_wl, (k_h2h, blk)], [psk(pl)])
                                    g_, k_g = eg[s_]
                                    sg_, k_sg = es_[s_]
                                    l_, k_l = el[s_]
                                    a_, k_a = ea[s_]
                                    ts("dve", g_, PS[pg][:, :], b1s[:, e_, fc:fc + 1], 7.0, ALU.add, ALU.min,
                                       [psk(pg), k_b1s], [k_g])
                                    act(sg_, g_, AF.Sigmoid, [k_g], [k_sg], scale=1.702)
                                    act(l_, PS[pl][:, :], AF.Identity, [psk(pl), k_b1s], [k_l], bias=b1s[:, e_, 16 + fc:17 + fc])
                                    ts("pool", l_, l_, -7.0, 7.0, ALU.max, ALU.min, [k_l], [k_l])
                                    tt("pool", a_, g_, sg_, ALU.mult, [k_g, k_sg], [k_a])
                                    stt(actT[:, fl, blk * 512:(blk + 1) * 512], l_, 1.0, a_, ALU.add, ALU.mult,
                                        [k_l, k_a], [(k_actT, fl, blk)])
                        for db in range(4):
                            w2_, k_w2 = w2b[wi2 % 2]
                            k2 = f"w2b{wi2 % 2}"
                            wi2 += 1
                            load_w(w2_[:, :, :], w2_v[e_][:, fh * 8:(fh + 1) * 8, db * 512:(db + 1) * 512], k_w2, k2)
                            for tl in range(8):
                                t_ = half * 8 + tl
                                pb = 4 + tl % 4
                                for fl in range(8):
                                    mm(PS[pb][:, :], actT[:, fl, tl * 128:(tl + 1) * 128], w2_[:, fl, :], fl == 0, fl == 7,
                                       [(k_actT, fl, tl // 4), k_w2], [psk(pb)])
                                stt(yacc[:, tl, db * 512:(db + 1) * 512], PS[pb][:, :], G_sb[:, t_, e_:e_ + 1],
                                    yacc[:, tl, db * 512:(db + 1) * 512], ALU.mult, ALU.add,
                                    [psk(pb), (k_G, t_), (k_yacc, tl, db)], [(k_yacc, tl, db)])
                for tl in range(8):
                    t_ = half * 8 + tl
                    yk = [(k_yacc, tl, db) for db in range(4)]
                    jk = [(k_actT, fl_, b__) for fl_ in range(2) for b__ in range(2)]
                    x1t = epi_t[:, 0:4, :].rearrange("p a b -> p (a b)")
                    k_x1l = [(k_epi, i_) for i_ in range(4)]
                    act(actT[:, 0:2, :].rearrange("p a b -> p (a b)"), yacc[:, tl, :], AF.Square, yk, jk + [k_ssqD], accum=ssqD[:])
                    rstd_from_ssq(ssqD[:], k_ssqD)
                    dma("sp", x1t, x1_d[t_ * 128:(t_ + 1) * 128, :], [("x1_d", t_)], k_x1l, "c_x1t")
                    stt(yacc[:, tl, :], yacc[:, tl, :], ssqD[:, 0:1], gt2g[:], ALU.mult, ALU.mult, yk + [k_ssqD, k_gt2g], yk)
                    tt("pool", x1t, x1t, yacc[:, tl, :], ALU.add, k_x1l + yk, k_x1l)
                    dma("sp", out[t_ * 128:(t_ + 1) * 128, :], x1t, k_x1l, [("out", t_)], "c_out")
            A.release(mD)

        except _Stop:
            pass
        S.emit(nc, st, final_waits=list(S.dma_counts.keys()))
    return nc


def _consts():
    t = np.arange(T, dtype=np.float32)

    def tabs(rot_dim, head_dim):
        half = rot_dim // 2
        inv = (np.float32(500000.0) ** (-np.arange(0, rot_dim, 2, dtype=np.float32) / np.float32(rot_dim))).astype(np.float32)
        ang = (t[:, None] * inv[None, :]).astype(np.float32)
        cos = np.cos(ang).astype(np.float32).T
        sin = np.sin(ang).astype(np.float32).T
        C = np.ones((128, T), np.float32)
        Sg = np.zeros((128, T), np.float32)
        Pm = np.zeros((128, 128), np.float32)
        for hb in range(0, 128, head_dim):
            C[hb:hb + half] = cos
            C[hb + half:hb + rot_dim] = cos
            Sg[hb:hb + half] = -sin
            Sg[hb + half:hb + rot_dim] = sin
            for i in range(half):
                Pm[hb + half + i, hb + i] = 1.0
                Pm[hb + i, hb + half + i] = 1.0
        return np.stack([C, Sg]), Pm

    rA, pA = tabs(32, 128)
    rI, pI = tabs(16, 64)
    tri = np.where(np.arange(128)[None, :] <= np.arange(128)[:, None], 0.0, -1.0e4).astype(np.float32)
    pinv = np.zeros((128, 4, 16), np.float32)
    for g, w in enumerate((2, 4, 8, 16)):
        pinv[:, g, :] = 1.0 / np.minimum(np.arange(16) + 1, w).astype(np.float32)
    return dict(identf=np.eye(128, dtype=np.float32), ropeA=rA, ropeI=rI,
                perms=np.stack([pA, pI]).astype(np.float32), trineg=tri, poolinv=pinv)


def make_in_maps(x, c, w_ada, b_ada, g_pre_mix, g_post_mix, w_in, w_pool_grp, pool_scale, w_up_pool, w_up_attn,
                 w_out, g_pre_ffn, g_post_ffn, w_router, b_router, w1, b1, w2, b2):
    f = lambda a: np.ascontiguousarray(np.asarray(a, dtype=np.float32))
    x = f(x)
    c = f(c)
    shared = dict(
        w_ada=f(w_ada)[0], b_ada=f(b_ada)[0][None, :],
        gvec=np.ascontiguousarray(np.stack([f(g_pre_mix)[0], f(g_post_mix)[0], f(g_pre_ffn)[0], f(g_post_ffn)[0]])),
        w_in=f(w_in)[0], w_pool=f(w_pool_grp)[0],
        pscale=np.ascontiguousarray(f(pool_scale)[0].reshape(8, 128).T),
        w_up_pool=f(w_up_pool)[0], w_up_attn=f(w_up_attn)[0], w_out=f(w_out)[0],
        w_router=f(w_router)[0], b_router=f(b_router)[0][None, :],
        w1=f(w1)[0], b1T=np.ascontiguousarray(f(b1)[0].reshape(NEXP, 32, 128).transpose(2, 0, 1)),
        w2=f(w2)[0], b2=f(b2)[0],
    )
    shared.update(_consts())
    maps = []
    for b in range(8):
        m = dict(shared)
        m["x"] = np.ascontiguousarray(x[b])
        m["cT"] = np.ascontiguousarray(c[b].reshape(16, 128).T)
        maps.append(m)
    return maps


def kernel(**inputs):
    in_maps = make_in_maps(**inputs)
    nc = build(dbg=False)
    res = run_bass_kernel_spmd(nc, in_maps, core_ids=list(range(8)))
    return np.stack([np.asarray(r["out"], dtype=np.float32) for r in res.results], axis=0)
```

```python
import numpy as np
from contextlib import ExitStack
import concourse.bass as bass
import concourse.mybir as mybir
from concourse.bass_utils import run_bass_kernel_spmd

F32 = mybir.dt.float32
BF16 = mybir.dt.bfloat16
AF = mybir.ActivationFunctionType
ALU = mybir.AluOpType
AX = mybir.AxisListType

D = 2048
T = 2048
NT = 16
NEXP = 32
EPS = 1e-6
ENGS = ("pe", "act", "dve", "pool", "sp")
CAP = 30000
SB_BASE = 16512
SB_END = 229376


class Op:
    __slots__ = ("eng", "fn", "reads", "writes", "dma_key", "ordinal", "waits", "signals",
                 "sigidx", "dma_cnt", "clock", "dma_clock", "alias")


def kname(k):
    return k[0] if isinstance(k, tuple) else k


class Sched:
    def __init__(self):
        self.ops = []
        self.per_eng = {e: [] for e in ENGS}
        self.dma_counts = {}

    def op(self, eng, fn, reads=(), writes=(), dma_key=None):
        o = Op()
        o.eng = eng
        o.fn = fn
        def _flat(xs):
            r = []
            for x_ in xs:
                if isinstance(x_, list):
                    r.extend(_flat(x_))
                else:
                    r.append(x_)
            return tuple(r)
        o.reads = _flat(reads)
        o.writes = _flat(writes)
        o.dma_key = dma_key
        o.signals = False
        o.sigidx = 0
        o.waits = []
        o.alias = None
        o.dma_cnt = 0
        o.ordinal = len(self.per_eng[eng])
        if dma_key is not None:
            self.dma_counts[dma_key] = self.dma_counts.get(dma_key, 0) + 1
            o.dma_cnt = self.dma_counts[dma_key]
        self.per_eng[eng].append(o)
        self.ops.append(o)
        return o

    def alias(self, new_name, old_names):
        o = Op()
        o.eng = None
        o.alias = (new_name, tuple(old_names))
        self.ops.append(o)

    def resolve(self):
        last_writer = {}
        readers = {}
        by_name = {}
        inherit = {}
        clock = {e: {} for e in ENGS}
        dclock = {e: {} for e in ENGS}

        def touch(k):
            if k not in readers:
                n = kname(k)
                readers[k] = list(inherit.get(n, ()))
                by_name.setdefault(n, []).append(k)

        for o in self.ops:
            if o.eng is None:
                new, olds = o.alias
                lst = inherit.setdefault(new, [])
                for on in olds:
                    lst.extend(inherit.get(on, ()))
                    for k in by_name.get(on, ()):
                        w = last_writer.get(k)
                        if w is not None:
                            lst.append(w)
                        lst.extend(readers.get(k, ()))
                best = {}
                for d in lst:
                    kk = ("d", d.dma_key) if d.dma_key is not None else ("e", d.eng)
                    val = d.dma_cnt if d.dma_key is not None else d.ordinal
                    if kk not in best or best[kk][0] < val:
                        best[kk] = (val, d)
                inherit[new] = [v[1] for v in best.values()]
                continue
            deps = []
            for r in o.reads:
                touch(r)
                w = last_writer.get(r)
                if w is not None:
                    deps.append(w)
                if kname(r) == "ps":
                    for k2 in by_name.get("ps", ()):
                        if k2[1] == r[1]:
                            deps.extend(o2 for o2 in readers[k2] if o2.eng != o.eng)
            for wk in o.writes:
                touch(wk)
                w = last_writer.get(wk)
                if w is not None:
                    deps.append(w)
                deps.extend(readers[wk])
            ck = clock[o.eng]
            dk = dclock[o.eng]
            need_e = {}
            need_d = {}
            for d in deps:
                if d is o:
                    continue
                if d.dma_key is not None:
                    if dk.get(d.dma_key, 0) >= d.dma_cnt:
                        continue
                    if need_d.get(d.dma_key, (0, None))[0] < d.dma_cnt:
                        need_d[d.dma_key] = (d.dma_cnt, d)
                else:
                    if d.eng == o.eng and o.eng == "pe" and o.dma_key is None:
                        continue
                    if ck.get(d.eng, -1) >= d.ordinal:
                        continue
                    if need_e.get(d.eng, (-1, None))[0] < d.ordinal:
                        need_e[d.eng] = (d.ordinal, d)
            waits = []
            for e, (ordn, d) in need_e.items():
                d.signals = True
                waits.append(("e", d))
                ck[e] = max(ck.get(e, -1), ordn)
                for e2, v in d.clock.items():
                    if e2 != o.eng and ck.get(e2, -1) < v:
                        ck[e2] = v
                for k2, v in d.dma_clock.items():
                    if dk.get(k2, 0) < v:
                        dk[k2] = v
            for k, (cnt, d) in need_d.items():
                waits.append(("d", d))
                dk[k] = max(dk.get(k, 0), cnt)
                for e2, v in d.clock.items():
                    if e2 != o.eng and ck.get(e2, -1) < v:
                        ck[e2] = v
                for k2, v in d.dma_clock.items():
                    if dk.get(k2, 0) < v:
                        dk[k2] = v
            o.waits = waits
            o.clock = dict(ck)
            o.dma_clock = dict(dk)
            for r in o.reads:
                readers[r].append(o)
            for wk in o.writes:
                last_writer[wk] = o
                readers[wk] = []
        self.nsig = {}
        for e in ENGS:
            n = 0
            for o in self.per_eng[e]:
                if o.dma_key is None and o.signals:
                    n += 1
                    o.sigidx = n
            self.nsig[e] = n

    def emit(self, nc, stack, final_waits=()):
        self.resolve()
        esems = {}
        for e in ENGS:
            nep = (self.nsig[e] + CAP - 1) // CAP
            esems[e] = [stack.enter_context(nc.semaphore(f"s_{e}_{i}")) for i in range(nep)]
        dsems = {}
        for i, k in enumerate(self.dma_counts):
            dsems[k] = stack.enter_context(nc.semaphore(f"d_{i}"))
        block = stack.enter_context(nc.Block())

        def run(e, engobj):
            for o in self.per_eng[e]:
                for kind, d in o.waits:
                    if kind == "e":
                        ep, val = (d.sigidx - 1) // CAP, (d.sigidx - 1) % CAP + 1
                        engobj.wait_ge(esems[d.eng][ep], val)
                    else:
                        engobj.wait_ge(dsems[d.dma_key], 16 * d.dma_cnt)
                ins = o.fn(engobj)
                if o.dma_key is not None:
                    ins.then_inc(dsems[o.dma_key], 16)
                elif o.signals:
                    ep = (o.sigidx - 1) // CAP
                    ins.then_inc(esems[e][ep], 1)
            if e == "sp":
                for k in final_waits:
                    engobj.wait_ge(dsems[k], 16 * self.dma_counts[k])

        @block.tensor
        def _(eng):
            run("pe", eng)

        @block.scalar
        def _(eng):
            run("act", eng)

        @block.vector
        def _(eng):
            run("dve", eng)

        @block.gpsimd
        def _(eng):
            run("pool", eng)

        @block.sync
        def _(eng):
            run("sp", eng)


class Arena:
    def __init__(self, nc, S):
        self.nc = nc
        self.S = S
        self.top = SB_BASE
        self.live = []
        self.freed = []
        self.cnt = 0

    def alloc(self, name, shape, dtype):
        esz = 4 if dtype == F32 else 2
        n = 1
        for s in shape[1:]:
            n *= s
        nbytes = (n * esz + 63) // 64 * 64
        start = self.top
        end = start + nbytes
        assert end <= SB_END, f"SBUF overflow allocating {name}: {end}"
        self.cnt += 1
        uname = f"{name}_{self.cnt}"
        h = self.nc.alloc_sbuf_tensor_at(uname, list(shape), dtype, offset=start)
        olds = [f[2] for f in self.freed if f[0] < end and start < f[1]]
        if olds:
            self.S.alias(uname, olds)
        self.live.append((start, end, uname))
        self.top = end
        return h, uname

    def mark(self):
        return (self.top, len(self.live))

    def release(self, m):
        top, n = m
        for rec in self.live[n:]:
            self.freed.append(rec)
        del self.live[n:]
        self.top = top


class _Stop(Exception):
    pass


def build(dbg=False, stop=None):
    nc = bass.Bass("TRN2", target_bir_lowering=False)
    S = Sched()

    def din(name, shape, dt=F32):
        return nc.dram_tensor(name, list(shape), dt, kind="ExternalInput").ap()

    x = din("x", [T, D])
    cT = din("cT", [128, 16])
    w_ada = din("w_ada", [D, 6 * D])
    b_ada = din("b_ada", [1, 6 * D])
    gvec = din("gvec", [4, D])
    w_in = din("w_in", [D, 9296])
    w_pool = din("w_pool", [4, 256, 256])
    pscale = din("pscale", [128, 8])
    w_up_pool = din("w_up_pool", [1024, D])
    w_up_attn = din("w_up_attn", [D, D])
    w_out = din("w_out", [D, D])
    w_router = din("w_router", [D, NEXP])
    b_router = din("b_router", [1, NEXP])
    w1 = din("w1", [NEXP, D, 2 * D] if stop is None else [1, 128, 128])
    b1T = din("b1T", [128, NEXP, 32])
    w2 = din("w2", [NEXP, D, D] if stop is None else [1, 128, 128])
    b2 = din("b2", [NEXP, D])
    identf = din("identf", [128, 128])
    ropeA = din("ropeA", [2, 128, T])
    ropeI = din("ropeI", [2, 128, T])
    perms = din("perms", [2, 128, 128])
    trineg = din("trineg", [128, 128])
    poolinv = din("poolinv", [128, 4, 16])

    kind_s = "ExternalOutput" if dbg else "Internal"
    out = nc.dram_tensor("out", [T, D], F32, kind="ExternalOutput").ap()
    modrow = nc.dram_tensor("modrow", [6, D], F32, kind=kind_s).ap()
    hT_d = nc.dram_tensor("hT_d", [128, 16, T], BF16, kind=kind_s).ap()
    ypT_d = nc.dram_tensor("ypT_d", [128, 8, T], BF16, kind=kind_s).ap()
    oT_d = nc.dram_tensor("oT_d", [128, 16, T], BF16, kind=kind_s).ap()
    x1_d = nc.dram_tensor("x1_d", [T, D], F32, kind=kind_s).ap()
    h2T_d = nc.dram_tensor("h2T_d", [128, 16, T], BF16, kind=kind_s).ap()
    G_d = nc.dram_tensor("G_d", [128, NT, NEXP], F32, kind=kind_s).ap()
    mk_d = nc.dram_tensor("mk_d", [T, T], BF16, kind=kind_s).ap() if dbg else None

    with ExitStack() as st:
        A = Arena(nc, S)
        PS = [st.enter_context(nc.psum_tensor(f"psb{i}", [128, 512], F32)) for i in range(8)]
        PSB = [p.bitcast(BF16) for p in PS]

        def psk(b, sub=None):
            return ("ps", b) if sub is None else ("ps", b, sub)

        def dma(eng, out_ap, in_ap, reads, writes, key):
            S.op(eng, lambda e: e.dma_start(out=out_ap, in_=in_ap), reads, writes, dma_key=key)

        def mm(out_ap, lhsT, rhs, start, stop, reads, writes):
            S.op("pe", lambda e: e.matmul(out_ap, lhsT=lhsT, rhs=rhs, start=start, stop=stop), reads, writes)

        def tr(out_ap, in_ap, ident, reads, writes):
            S.op("pe", lambda e: e.transpose(out=out_ap, in_=in_ap, identity=ident), reads, writes)

        def act(out_ap, in_ap, func, reads, writes, bias=None, scale=None, accum=None):
            kw = {}
            if bias is not None:
                kw["bias"] = bias
            if scale is not None:
                kw["scale"] = scale
            if accum is not None:
                kw["accum_out"] = accum
            S.op("act", lambda e: e.activation(out=out_ap, in_=in_ap, func=func, **kw), reads, writes)

        def ts(eng, out_ap, in0, s1, s2, op0, op1, reads, writes, accum=None):
            if op1 is None:
                S.op(eng, lambda e: e.tensor_scalar(out=out_ap, in0=in0, scalar1=s1, scalar2=None, op0=op0), reads, writes)
            elif accum is None:
                S.op(eng, lambda e: e.tensor_scalar(out=out_ap, in0=in0, scalar1=s1, scalar2=s2, op0=op0, op1=op1), reads, writes)
            else:
                S.op(eng, lambda e: e.tensor_scalar(out=out_ap, in0=in0, scalar1=s1, scalar2=s2, op0=op0, op1=op1, accum_out=accum), reads, writes)

        def tt(eng, out_ap, in0, in1, op, reads, writes):
            S.op(eng, lambda e: e.tensor_tensor(out=out_ap, in0=in0, in1=in1, op=op), reads, writes)

        def stt(out_ap, in0, scalar, in1, op0, op1, reads, writes):
            S.op("dve", lambda e: e.scalar_tensor_tensor(out=out_ap, in0=in0, scalar=scalar, in1=in1, op0=op0, op1=op1), reads, writes)

        def cp(eng, out_ap, in_ap, reads, writes):
            if eng == "act":
                S.op("act", lambda e: e.copy(out=out_ap, in_=in_ap), reads, writes)
            else:
                S.op(eng, lambda e: e.tensor_copy(out=out_ap, in_=in_ap), reads, writes)

        def bcast_row(dram_ap_row, n):
            return bass.AP(tensor=dram_ap_row.tensor, offset=dram_ap_row.offset, ap=[[0, 128], [1, n]])

        def rstd_from_ssq(ssq, nm):
            ts("dve", ssq, ssq, 1.0 / D, EPS, ALU.mult, ALU.add, [nm], [nm])
            act(ssq, ssq, AF.Sqrt, [nm], [nm])
            S.op("dve", lambda e: e.reciprocal(out=ssq, in_=ssq), [nm], [nm])

        idf, k_idf = A.alloc("identf", [128, 128], F32)
        idb, k_idb = A.alloc("identb", [128, 128], BF16)
        dma("sp", idf[:], identf[:, :], [], [k_idf], "c_idf")
        dma("pool", idb[:], identf[:, :], [], [k_idb], "c_idb")
        G_sb, k_G = A.alloc("G", [128, NT, NEXP], F32)

        w_in_v = w_in.rearrange("(kc p) n -> p kc n", p=128)

        def chk(name):
            if stop == name:
                raise _Stop()

        try:
            mA = A.mark()
            cact, k_cact = A.alloc("cact", [128, 16], F32)
            crep, k_crep = A.alloc("crep", [128, 16, 128], F32)
            modbc = []
            for j in range(6):
                modbc.append(A.alloc(f"modbc{j}", [128, D], F32))
            gbc, k_gbc = A.alloc("gbc", [128, D], F32)
            babc, k_babc = A.alloc("babc", [128, D], F32)
            wa = [A.alloc(f"wa{i}", [128, 16, 512], F32) for i in range(2)]
            dma("sp", cact[:], cT[:, :], [], [k_cact], "c_cact")
            act(cact[:], cact[:], AF.Silu, [k_cact], [k_cact])
            for kc in range(16):
                cp("dve", crep[:, kc, :], cact[:, kc:kc + 1].to_broadcast([128, 128]), [k_cact], [(k_crep, kc)])
            w_ada_v = w_ada.rearrange("(kc p) n -> p kc n", p=128)
            it = 0
            for j in range(6):
                dma("sp", babc[:], bcast_row(b_ada[0:1, j * D:(j + 1) * D], D), [], [k_babc], "c_babc")
                for cb in range(4):
                    slot = it % 2
                    wt, k_wt = wa[slot]
                    c0 = j * D + cb * 512
                    dma("sp", wt[:, :, :], w_ada_v[:, :, c0:c0 + 512], [], [k_wt], f"wa{slot}")
                    pb = it % 2
                    for kc in range(16):
                        mm(PS[pb][:, :], crep[:, kc, :], wt[:, kc, :], kc == 0, kc == 15,
                           [(k_crep, kc), k_wt], [psk(pb)])
                    tt("dve", modbc[j][0][:, cb * 512:(cb + 1) * 512], PS[pb][:, :], babc[:, cb * 512:(cb + 1) * 512],
                       ALU.add, [psk(pb), k_babc], [(modbc[j][1], cb)])
                    it += 1
            def allk(j):
                return [(modbc[j][1], cb) for cb in range(4)]

            def derive(jmod, grow, add_one, outrow):
                dma("sp", gbc[:], bcast_row(gvec[grow:grow + 1, :], D), [], [k_gbc], "c_gbc")
                if add_one:
                    stt(modbc[jmod][0][:, :], modbc[jmod][0][:, :], 1.0, gbc[:, :], ALU.add, ALU.mult,
                        allk(jmod) + [k_gbc], allk(jmod))
                else:
                    tt("dve", modbc[jmod][0][:, :], modbc[jmod][0][:, :], gbc[:, :], ALU.mult,
                       allk(jmod) + [k_gbc], allk(jmod))
                dma("sp", modrow[outrow:outrow + 1, :], modbc[jmod][0][0:1, :], allk(jmod), [("modrow", outrow)], f"modrow{outrow}")

            derive(1, 0, True, 0)
            dma("sp", modrow[1:2, :], modbc[0][0][0:1, :], allk(0), [("modrow", 1)], "modrow1")
            derive(2, 1, False, 2)
            derive(4, 2, True, 3)
            dma("sp", modrow[4:5, :], modbc[3][0][0:1, :], allk(3), [("modrow", 4)], "modrow4")
            derive(5, 3, False, 5)
            A.release(mA)
            chk("A")

            def load_bc(dst, k_dst, row, key):
                dma("sp", dst[:], bcast_row(modrow[row:row + 1, :], D), [("modrow", row)], [k_dst], key)

            mB = A.mark()
            hT, k_hT = A.alloc("hT", [128, 16, T], BF16)
            mB2 = A.mark()
            A1, k_A1 = A.alloc("A1", [128, D], F32)
            sh1, k_sh1 = A.alloc("sh1", [128, D], F32)
            load_bc(A1, k_A1, 0, "c_A1")
            load_bc(sh1, k_sh1, 1, "c_sh1")
            xt = [A.alloc(f"xt{i}", [128, D], F32) for i in range(2)]
            hb = [A.alloc(f"hb{i}", [128, D], BF16) for i in range(2)]
            junk, k_junk = A.alloc("junkB", [128, D], BF16)
            ssq = [A.alloc(f"ssq{i}", [128, 1], F32) for i in range(2)]

            def norm_mod_tile(src, k_src, dst, k_dst, Abc, k_Abc, shbc, k_shbc, ssq_t, k_ssq, junk_t, k_junk_t):
                act(junk_t[:], src[:], AF.Square, [k_src], [k_junk_t, k_ssq], accum=ssq_t[:])
                rstd_from_ssq(ssq_t[:], k_ssq)
                stt(src[:], src[:], ssq_t[:, 0:1], Abc[:], ALU.mult, ALU.mult, [k_src, k_ssq, k_Abc], [k_src])
                tt("pool", dst[:], src[:], shbc[:], ALU.add, [k_src, k_shbc], [k_dst])

            for t_ in range(NT):
                s_ = t_ % 2
                xt_, k_xt = xt[s_]
                hb_, k_hb = hb[s_]
                dma("sp", xt_[:], x[t_ * 128:(t_ + 1) * 128, :], [], [k_xt], f"xt{s_}")
                norm_mod_tile(xt_, k_xt, hb_, k_hb, A1, k_A1, sh1, k_sh1, ssq[s_][0], ssq[s_][1], junk, k_junk)
                b0 = 2 * s_
                for kc in range(16):
                    bb = b0 + kc // 8
                    tr(PSB[bb][:, (kc % 8) * 128:(kc % 8 + 1) * 128], hb_[:, kc * 128:(kc + 1) * 128], idb[:],
                       [k_hb, k_idb], [psk(bb)])
                for hh in range(2):
                    cp("act" if hh == 0 else "dve", hT[:, hh * 8:(hh + 1) * 8, t_ * 128:(t_ + 1) * 128],
                       PSB[b0 + hh][:, :].rearrange("p (a b) -> p a b", a=8), [psk(b0 + hh)], [(k_hT, t_)])
            A.release(mB2)
            for hh in range(4):
                dma("sp", hT_d[:, :, hh * 512:(hh + 1) * 512], hT[:, :, hh * 512:(hh + 1) * 512],
                    [(k_hT, t_) for t_ in range(hh * 4, hh * 4 + 4)], [("hT_d", hh)], f"hT_d{hh}")
            hT_keys = [(k_hT, t_) for t_ in range(NT)]
            chk("B")

            def hT_blk_keys(blk):
                return [(k_hT, t_) for t_ in range(blk * 4, blk * 4 + 4)]

            def load_w(dst_ap, src_ap, k_dst, key):
                dma("pool", dst_ap, src_ap, [], [k_dst], key)

            mC3 = A.mark()
            wb = [A.alloc(f"wbP{i}", [128, 16, 512], BF16) for i in range(2)]
            wgrp, k_wgrp = A.alloc("wgrp", [128, 4, 2, 256], BF16)
            psc, k_psc = A.alloc("psc", [128, 8], F32)
            pinv, k_pinv = A.alloc("pinv", [128, 4, 16], F32)
            ub = [A.alloc(f"ub{i}", [128, 16 + T], F32) for i in range(3)]
            mixT, k_mixT = A.alloc("mixT", [128, 8, T], BF16)
            t16, k_t16 = A.alloc("t16", [128, 16], F32)
            ypst = [A.alloc(f"ypst{i}", [128, 512], BF16) for i in range(2)]
            for g in range(4):
                load_w(wgrp[:, g, :, :], w_pool[g].rearrange("(cc p) d -> p cc d", p=128), (k_wgrp, g), f"c_wgrp{g}")
            dma("sp", psc[:], pscale[:, :], [], [k_psc], "c_psc")
            dma("sp", pinv[:, :, :], poolinv[:, :, :], [], [k_pinv], "c_pinv")
            for i in range(3):
                S.op("pool", lambda e, i=i: e.memset(ub[i][0][:, 0:16], 0.0), [], [(ub[i][1], "pad")])
            pbi = 0
            for grp in range(2):
                wt, k_wt = wb[grp % 2]
                load_w(wt[:, :, :], w_in_v[:, :, grp * 512:(grp + 1) * 512], k_wt, f"wbP{grp % 2}")
                for oc in range(4):
                    c = grp * 4 + oc
                    g = c // 2
                    u_, k_u = ub[0]
                    for blk in range(4):
                        pb = pbi % 2
                        pbi += 1
                        for kc in range(16):
                            mm(PS[pb][:, :], wt[:, kc, oc * 128:(oc + 1) * 128], hT[:, kc, blk * 512:(blk + 1) * 512],
                               kc == 0, kc == 15, [k_wt] + hT_blk_keys(blk), [psk(pb)])
                        cp("act", u_[:, 16 + blk * 512:16 + (blk + 1) * 512], PS[pb][:, :], [psk(pb)], [(k_u, blk)])
                    ukeys = [(k_u, b_) for b_ in range(4)] + [(k_u, "pad")]
                    cur, k_cur = u_, ukeys
                    dst_i = 1
                    d_ = 1
                    for step in range(g + 1):
                        nxt, k_nx = ub[dst_i]
                        tt("dve", nxt[:, 16:16 + T], cur[:, 16:16 + T], cur[:, 16 - d_:16 - d_ + T], ALU.add,
                           k_cur, [(k_nx, "all")])
                        cur, k_cur = nxt, [(k_nx, "all"), (k_nx, "pad")]
                        dst_i = 3 - dst_i
                        d_ *= 2
                    w_ = 2 ** (g + 1)
                    stt(mixT[:, c, :], cur[:, 16:16 + T], 1.0 / w_, u_[:, 16:16 + T], ALU.mult, ALU.subtract,
                        k_cur + ukeys, [(k_mixT, c)])
                    tt("dve", t16[:], cur[:, 16:32], pinv[:, g, :], ALU.mult, k_cur + [k_pinv], [k_t16])
                    tt("dve", mixT[:, c, 0:16], t16[:], u_[:, 16:32], ALU.subtract, [k_t16] + ukeys, [(k_mixT, c)])
            si = 0
            for g in range(4):
                for dd in range(2):
                    oc_ = g * 2 + dd
                    for blk in range(4):
                        pb = pbi % 2
                        pbi += 1
                        for cc in range(2):
                            mm(PS[pb][:, :], wgrp[:, g, cc, dd * 128:(dd + 1) * 128], mixT[:, g * 2 + cc, blk * 512:(blk + 1) * 512],
                               cc == 0, cc == 1, [(k_wgrp, g), (k_mixT, g * 2 + cc)], [psk(pb)])
                        ys, k_ys = ypst[si % 2]
                        si += 1
                        ts("dve", ys[:], PS[pb][:, :], psc[:, oc_:oc_ + 1], None, ALU.mult, None, [psk(pb), k_psc], [k_ys])
                        dma("sp", ypT_d[:, oc_, blk * 512:(blk + 1) * 512], ys[:], [k_ys], [("ypT_d", blk)], f"ypst{si % 2}")
            A.release(mC3)
            chk("C3")

            def rope(pb, rb, perm, k_perm, tab, k_tab, blk, raw_t, tmp1_t, tmp2_t, dst_ap, dst_keys, psl=slice(0, 128)):
                raw, k_raw = raw_t
                t1, k_t1 = tmp1_t
                t2, k_t2 = tmp2_t
                RM = 3
                if RM != 11:
                    cp("act", raw[:], PS[pb][:, :], [psk(pb)], [k_raw])
                if RM == 12:
                    cp("dve", t1[:], PS[pb][:, :], [psk(pb)], [k_t1])
                    return
                if RM == 11:
                    tt("dve", t1[:], PS[pb][:, :], tab[:, 0, blk * 512:(blk + 1) * 512], ALU.mult, [psk(pb), k_tab], [k_t1])
                    return
                if RM >= 2:
                    mm(PS[rb][:, :], perm, raw[:], True, True, [k_perm, k_raw], [psk(rb)])
                if RM == 0:
                    return
                tt("dve", t1[:], PS[pb][:, :], tab[:, 0, blk * 512:(blk + 1) * 512], ALU.mult, [psk(pb), k_tab], [k_t1])
                if RM == 10:
                    return
                if RM >= 3:
                    tt("dve", t2[:], PS[rb][:, :], tab[:, 1, blk * 512:(blk + 1) * 512], ALU.mult, [psk(rb), k_tab], [k_t2])
                    tt("pool", dst_ap, t1[psl, :], t2[psl, :], ALU.add, [k_t1, k_t2], dst_keys)
                else:
                    cp("pool", dst_ap, t1[psl, :], [k_t1], dst_keys)

            mC1 = A.mark()
            MOFF = []
            off = 0
            for j in range(NT):
                MOFF.append(off)
                off += T - 128 * j
            maskT, k_maskT = A.alloc("maskT", [128, off], BF16)
            mC1b = A.mark()
            qiT, k_qiT = A.alloc("qiT", [128, 8, T], BF16)
            kiT, k_kiT = A.alloc("kiT", [128, T], BF16)
            wi, k_wi = A.alloc("wi", [128, NT, 16], F32)
            wabs, k_wabs = A.alloc("wabs", [128, NT, 16], F32)
            wsgn, k_wsgn = A.alloc("wsgn", [128, NT, 16], F32)
            mC1c = A.mark()
            wb = [A.alloc(f"wbI{i}", [128, 16, 512], BF16) for i in range(2)]
            wkiA, k_wkiA = A.alloc("wkiA", [128, 16, 128], BF16)
            wkiB, k_wkiB = A.alloc("wkiB", [128, 16, 128], BF16)
            rtab, k_rtab = A.alloc("rtabI", [128, 2, T], F32)
            permI, k_permI = A.alloc("permI", [128, 128], BF16)
            raws = [A.alloc(f"rawI{i}", [128, 512], BF16) for i in range(2)]
            t1s = [A.alloc(f"t1I{i}", [128, 512], F32) for i in range(2)]
            t2s = [A.alloc(f"t2I{i}", [128, 512], F32) for i in range(2)]
            dma("sp", rtab[:, 0, :], ropeI[0], [], [(k_rtab, 0)], "c_rtabI0")
            dma("sp", rtab[:, 1, :], ropeI[1], [], [(k_rtab, 1)], "c_rtabI1")
            k_rt = [(k_rtab, 0), (k_rtab, 1)]
            dma("pool", permI[:], perms[1], [], [k_permI], "c_permI")
            load_w(wkiA[:, :, :], w_in_v[:, :, 5056:5184], k_wkiA, "c_wki0")
            load_w(wkiB[:, :, :], w_in_v[:, :, 5120:5248], k_wkiB, "c_wki1")
            ri = 0
            chk("C1a0")
            for grp in range(2):
                if grp == 1:
                    chk("C1a1")
                wt, k_wt = wb[grp % 2]
                load_w(wt[:, :, :], w_in_v[:, :, 4096 + grp * 512:4096 + (grp + 1) * 512], k_wt, f"wbI{grp % 2}")
                for oc in range(4):
                    c = grp * 4 + oc
                    for blk in range(4):
                        pb = ri % 2
                        for kc in range(16):
                            mm(PS[pb][:, :], wt[:, kc, oc * 128:(oc + 1) * 128], hT[:, kc, blk * 512:(blk + 1) * 512],
                               kc == 0, kc == 15, [k_wt] + hT_blk_keys(blk), [psk(pb)])
                        rope(pb, 2 + pb, permI[:], k_permI, rtab, k_rt[0:2], blk, raws[pb], t1s[pb], t2s[pb],
                             qiT[:, c, blk * 512:(blk + 1) * 512], [(k_qiT, c, blk)])
                        ri += 1
            for blk in range(4):
                for (wk__, k_wk__, psl_, hf_) in ((wkiA, k_wkiA, slice(64, 128), 1), (wkiB, k_wkiB, slice(0, 64), 0)):
                    pb = ri % 2
                    for kc in range(16):
                        mm(PS[pb][:, :], wk__[:, kc, :], hT[:, kc, blk * 512:(blk + 1) * 512], kc == 0, kc == 15,
                           [k_wk__] + hT_blk_keys(blk), [psk(pb)])
                    rope(pb, 2 + pb, permI[:], k_permI, rtab, k_rt, blk, raws[pb], t1s[pb], t2s[pb],
                         kiT[psl_, blk * 512:(blk + 1) * 512], [(k_kiT, blk, hf_)], psl=psl_)
                    ri += 1
            chk("C1a2")
            for t_ in range(NT):
                pb = 4 + t_ % 2
                for kc in range(16):
                    mm(PS[pb][:, 0:16], hT[:, kc, t_ * 128:(t_ + 1) * 128], wkiB[:, kc, 64:80], kc == 0, kc == 15,
                       [k_wkiB, (k_hT, t_)], [psk(pb)])
                ts("dve", wi[:, t_, :], PS[pb][:, 0:16], 1.0 / 32.0, None, ALU.mult, None, [psk(pb)], [(k_wi, t_)])
            wi_keys = [(k_wi, t_) for t_ in range(NT)]
            chk("C1a3")
            act(wsgn[:, :, :], wi[:, :, :], AF.Sign, wi_keys, [k_wsgn])
            tt("dve", wabs[:, :, :], wi[:, :, :], wsgn[:, :, :], ALU.mult, wi_keys + [k_wsgn], [k_wabs])
            A.release(mC1c)
            chk("C1a")
            sc = [A.alloc(f"sc{i}", [128, T], F32) for i in range(2)]
            rr = [A.alloc(f"rr{i}", [128, 512], F32) for i in range(4)]
            mk = [A.alloc(f"mk{i}", [128, T], BF16) for i in range(2)]
            jnk, k_jnk = A.alloc("jnkI", [128, T], BF16)
            tneg, k_tneg = A.alloc("tneg", [128, 128], F32)
            sm = [A.alloc(f"sm{i}", [128, 8], F32) for i in range(2)]
            dma("sp", tneg[:], trineg[:, :], [], [k_tneg], "c_tneg")
            NIT = 22
            rri = 0
            qi_keys_c = lambda c, i: [(k_qiT, c, i // 4)]
            for i in range(NT):
                L = 128 * (i + 1)
                nsb = (L + 511) // 512
                s0, k_s0 = sc[0]
                s1, k_s1 = sc[1]
                for h in range(16):
                    c, half = h // 2, h % 2
                    base = 64 * half
                    acc, k_acc = (s0, k_s0) if half == 0 else (s1, k_s1)
                    for sb in range(nsb):
                        n = min(512, L - 512 * sb)
                        pb = rri % 4
                        r_, k_r = rr[rri % 4]
                        rri += 1
                        mm(PS[pb][:, 0:n], qiT[base:base + 64, c, i * 128:(i + 1) * 128], kiT[base:base + 64, sb * 512:sb * 512 + n],
                           True, True, [(k_qiT, c, i // 4), (k_kiT, sb, half)], [psk(pb)])
                        act(r_[:, 0:n], PS[pb][:, 0:n], AF.Relu, [psk(pb), k_wabs], [k_r], scale=wabs[:, i, h:h + 1])
                        if h < 2:
                            ts("dve", acc[:, sb * 512:sb * 512 + n], r_[:, 0:n], wsgn[:, i, h:h + 1], None, ALU.mult, None,
                               [k_r, k_wsgn], [(k_acc, sb)])
                        else:
                            stt(acc[:, sb * 512:sb * 512 + n], r_[:, 0:n], wsgn[:, i, h:h + 1], acc[:, sb * 512:sb * 512 + n],
                                ALU.mult, ALU.add, [k_r, k_wsgn, (k_acc, sb)], [(k_acc, sb)])
                sk0 = [(k_s0, sb) for sb in range(nsb)]
                sk1 = [(k_s1, sb) for sb in range(nsb)]
                tt("pool", s0[:, 0:L], s0[:, 0:L], s1[:, 0:L], ALU.add, sk0 + sk1, sk0)
                sm_, k_sm = sm[i % 2]
                mk_, k_mk = mk[i % 2]
                if i >= 2:
                    S.op("dve", lambda e, s0=s0, sm_=sm_, L=L: e.tensor_reduce(out=sm_[:, 5:6], in_=s0[:, 0:L], axis=AX.X, op=ALU.max),
                         sk0, [(k_sm, 5)])
                    S.op("dve", lambda e, s0=s0, sm_=sm_, L=L: e.tensor_reduce(out=sm_[:, 0:1], in_=s0[:, 0:L], axis=AX.X, op=ALU.min),
                         sk0, [(k_sm, 0)])
                    tt("dve", sm_[:, 1:2], sm_[:, 5:6], sm_[:, 0:1], ALU.subtract, [(k_sm, 5), (k_sm, 0)], [(k_sm, 1)])
                tt("dve", s0[:, L - 128:L], s0[:, L - 128:L], tneg[:], ALU.add, sk0 + [k_tneg], sk0)
                if i >= 2:
                    for it_ in range(NIT):
                        f = 2.0 ** (-(it_ + 1))
                        stt(sm_[:, 2:3], sm_[:, 1:2], f, sm_[:, 0:1], ALU.mult, ALU.add, [(k_sm, 1), (k_sm, 0)], [(k_sm, 2)])
                        ts("dve", jnk[:, 0:L], s0[:, 0:L], sm_[:, 2:3], 0.0, ALU.is_ge, ALU.add, sk0 + [(k_sm, 2)],
                           [k_jnk, (k_sm, 3)], accum=sm_[:, 3:4])
                        ts("dve", sm_[:, 4:5], sm_[:, 3:4], 255.5, f, ALU.is_ge, ALU.mult, [(k_sm, 3)], [(k_sm, 4)])
                        stt(sm_[:, 0:1], sm_[:, 4:5], sm_[:, 1:2], sm_[:, 0:1], ALU.mult, ALU.add,
                            [(k_sm, 4), (k_sm, 1), (k_sm, 0)], [(k_sm, 0)])
                    ts("dve", mk_[:, 0:L], s0[:, 0:L], sm_[:, 0:1], None, ALU.is_ge, None, sk0 + [(k_sm, 0)], [k_mk])
                else:
                    ts("dve", mk_[:, 0:L], s0[:, 0:L], -1.0e3, None, ALU.is_ge, None, sk0, [k_mk])
                if dbg:
                    dma("sp", mk_d[i * 128:(i + 1) * 128, 0:L], mk_[:, 0:L], [k_mk], [("mk_d", i)], f"mkd{i % 2}")
                for j in range(i + 1):
                    bb = 4 + (j // 8) + 2 * (i % 2)
                    tr(PSB[bb][:, (j % 8) * 128:(j % 8 + 1) * 128], mk_[:, j * 128:(j + 1) * 128], idb[:], [k_mk, k_idb], [psk(bb)])
                for j in range(i + 1):
                    bb = 4 + (j // 8) + 2 * (i % 2)
                    o_ = MOFF[j] + (i - j) * 128
                    cp("act" if j % 2 == 0 else "dve", maskT[:, o_:o_ + 128], PSB[bb][:, (j % 8) * 128:(j % 8 + 1) * 128],
                       [psk(bb)], [(k_maskT, j, i)])
            A.release(mC1b)
            chk("C1")

            mC2 = A.mark()
            wb = [A.alloc(f"wbA{i}", [128, 16, 512], BF16) for i in range(2)]
            wkv = [A.alloc(f"wkv{i}", [128, 16, 256], BF16) for i in range(1)]
            rtabA, k_rtabA = A.alloc("rtabA", [128, 2, T], F32)
            permA, k_permA = A.alloc("permA", [128, 128], BF16)
            raws = [A.alloc(f"rawA{i}", [128, 512], BF16) for i in range(2)]
            t1s = [A.alloc(f"t1A{i}", [128, 512], F32) for i in range(2)]
            t2s = [A.alloc(f"t2A{i}", [128, 512], F32) for i in range(2)]
            qT = [A.alloc(f"qT{i}", [128, 4, T], BF16) for i in range(1)]
            kT_, k_kT = A.alloc("kT", [128, T], BF16)
            vg, k_vg = A.alloc("vg", [128, NT, 130], BF16)
            ex = [A.alloc(f"ex{i}", [128, 512], BF16) for i in range(3)]
            pp = [A.alloc(f"pp{i}", [128, 512], BF16) for i in range(3)]
            rden = [A.alloc(f"rden{i}", [128, 1], F32) for i in range(2)]
            otm = [A.alloc(f"otm{i}", [128, 128], BF16) for i in range(2)]
            ost = [A.alloc(f"ost{i}", [128, 512], BF16) for i in range(2)]
            dma("sp", rtabA[:, 0, :], ropeA[0], [], [(k_rtabA, 0)], "c_rtabA0")
            dma("sp", rtabA[:, 1, :], ropeA[1], [], [(k_rtabA, 1)], "c_rtabA1")
            k_rtA = [(k_rtabA, 0), (k_rtabA, 1)]
            dma("pool", permA[:], perms[0], [], [k_permA], "c_permA")
            SCL = 128.0 ** -0.5
            ri = 0
            exi = 0
            osti = 0
            for g in range(4):
                wt, k_wt = wb[g % 2]
                wk_, k_wk = wkv[0]
                load_w(wt[:, :, :], w_in_v[:, :, 1024 + g * 512:1024 + (g + 1) * 512], k_wt, f"wbA{g % 2}")
                load_w(wk_[:, :, 0:128], w_in_v[:, :, 3072 + g * 128:3072 + (g + 1) * 128], (k_wk, 0), "wkv_a")
                load_w(wk_[:, :, 128:256], w_in_v[:, :, 3584 + g * 128:3584 + (g + 1) * 128], (k_wk, 1), "wkv_b")
                q_, k_q = qT[0]
                for r in range(4):
                    for blk in range(4):
                        pb = ri % 2
                        for kc in range(16):
                            mm(PS[pb][:, :], wt[:, kc, r * 128:(r + 1) * 128], hT[:, kc, blk * 512:(blk + 1) * 512],
                               kc == 0, kc == 15, [k_wt] + hT_blk_keys(blk), [psk(pb)])
                        rope(pb, 2 + pb, permA[:], k_permA, rtabA, k_rtA, blk, raws[pb], t1s[pb], t2s[pb],
                             q_[:, r, blk * 512:(blk + 1) * 512], [(k_q, r, blk)])
                        ri += 1
                for blk in range(4):
                    pb = ri % 2
                    for kc in range(16):
                        mm(PS[pb][:, :], wk_[:, kc, 0:128], hT[:, kc, blk * 512:(blk + 1) * 512], kc == 0, kc == 15,
                           [(k_wk, 0)] + hT_blk_keys(blk), [psk(pb)])
                    rope(pb, 2 + pb, permA[:], k_permA, rtabA, k_rtA, blk, raws[pb], t1s[pb], t2s[pb],
                         kT_[:, blk * 512:(blk + 1) * 512], [(k_kT, blk)])
                    ri += 1
                S.op("pool", lambda e: e.memset(vg[:, :, 128:130], 1.0), [], [(k_vg, "one")])
                for t_ in range(NT):
                    pb = ri % 2
                    ri += 1
                    for kc in range(16):
                        mm(PS[pb][:, 0:128], hT[:, kc, t_ * 128:(t_ + 1) * 128], wk_[:, kc, 128:256], kc == 0, kc == 15,
                           [(k_wk, 1), (k_hT, t_)], [psk(pb)])
                    cp("act", vg[:, t_, 0:128], PS[pb][:, 0:128], [psk(pb)], [(k_vg, t_)])
                for r in range(4):
                    h = 4 * g + r
                    for qb in range(4):
                        nj = 4 * qb + 4
                        for j in range(nj):
                            t_lo = max(qb * 512, j * 128)
                            n = qb * 512 + 512 - t_lo
                            pb = exi % 2
                            e_, k_e = ex[exi % 3]
                            p_, k_p = pp[exi % 3]
                            exi += 1
                            mm(PS[pb][:, 0:n], kT_[:, j * 128:(j + 1) * 128], q_[:, r, t_lo:t_lo + n], True, True,
                               [(k_kT, j // 4), (k_q, r, qb)], [psk(pb)])
                            act(e_[:, 0:n], PS[pb][:, 0:n], AF.Exp, [psk(pb)], [k_e], scale=SCL)
                            mo = MOFF[j] + (t_lo - 128 * j)
                            mkeys = [(k_maskT, j, i_) for i_ in range(t_lo // 128, t_lo // 128 + n // 128)]
                            tt("dve" if exi % 2 == 0 else "pool", p_[:, 0:n], e_[:, 0:n], maskT[:, mo:mo + n], ALU.mult,
                               [k_e] + mkeys, [k_p])
                            for ts_ in range((t_lo - qb * 512) // 128, 4):
                                col = (qb * 512 + ts_ * 128) - t_lo
                                ab = 4 + ts_
                                ac = 0
                                jlast = 4 * qb + ts_
                                mm(PS[ab][:, ac:ac + 129], p_[:, col:col + 128], vg[:, j, 0:129], j == 0, j == jlast,
                                   [k_p, (k_vg, j), (k_vg, "one")], [psk(ab)])
                        os_, k_os = ost[osti % 2]
                        osti += 1
                        for ts_ in range(4):
                            ab = 4 + ts_
                            ac = 0
                            rd, k_rd = rden[ts_ % 2]
                            om, k_om = otm[ts_ % 2]
                            S.op("dve", lambda e, rd=rd, ab=ab, ac=ac: e.reciprocal(out=rd[:], in_=PS[ab][:, ac + 128:ac + 129]),
                                 [psk(ab)], [k_rd])
                            ts("dve", om[:], PS[ab][:, ac:ac + 128], rd[:, 0:1], None, ALU.mult, None,
                               [psk(ab), k_rd], [k_om])
                            tb = 2 + ts_ % 2
                            tr(PSB[tb][:, 0:128], om[:], idb[:], [k_om, k_idb], [psk(tb)])
                            cp("act", os_[:, ts_ * 128:(ts_ + 1) * 128], PSB[tb][:, 0:128], [psk(tb)], [(k_os, ts_)])
                        dma("sp", oT_d[:, h, qb * 512:(qb + 1) * 512], os_[:], [(k_os, ts_) for ts_ in range(4)],
                            [("oT_d", qb)], f"ost{osti % 2}")
            A.release(mC2)
            A.release(mC1)
            A.release(mB)
            chk("C2")

            mC4 = A.mark()
            wup = [A.alloc(f"wup{i}", [128, 56, 128], BF16) for i in range(2)]
            hTb, k_hTb = A.alloc("hTb", [128, 16, 512], BF16)
            ypb, k_ypb = A.alloc("ypb", [128, 8, 512], BF16)
            oTb, k_oTb = A.alloc("oTb", [128, 16, 512], BF16)
            mrg, k_mrg = A.alloc("mrg", [128, 16, 512], BF16)
            wo = [A.alloc(f"wo{i}", [128, 16, 512], BF16) for i in range(2)]
            sg = [A.alloc(f"sg{i}", [128, 512], F32) for i in range(4)]
            xt1, k_xt1 = A.alloc("xt1", [128, D], F32)
            h2t, k_h2t = A.alloc("h2t", [128, D], F32)
            gt1g, k_gt1g = A.alloc("gt1g", [128, D], F32)
            A2, k_A2 = A.alloc("A2", [128, D], F32)
            sh2, k_sh2 = A.alloc("sh2", [128, D], F32)
            h2Tf, k_h2Tf = A.alloc("h2Tf", [128, 16, 128], F32)
            h2Tb, k_h2Tb = A.alloc("h2Tb", [128, 16, 128], BF16)
            junk4, k_junk4 = A.alloc("junk4", [128, D], BF16)
            ssq4 = [A.alloc(f"ssq4{i}", [128, 1], F32) for i in range(2)]
            ssp, k_ssp = A.alloc("ssp", [128, 4], F32)
            wr, k_wr = A.alloc("wr", [128, 16, NEXP], F32)
            brbc, k_brbc = A.alloc("brbc", [128, NEXP], F32)
            lg, k_lg = A.alloc("lg", [128, NEXP], F32)
            m8, k_m8 = A.alloc("m8", [128, 8], F32)
            sel, k_sel = A.alloc("sel", [128, NEXP], F32)
            ee, k_ee = A.alloc("ee", [128, NEXP], F32)
            ssm, k_ssm = A.alloc("ssm", [128, 2], F32)
            load_bc(gt1g, k_gt1g, 2, "c_gt1g")
            load_bc(A2, k_A2, 3, "c_A2")
            load_bc(sh2, k_sh2, 4, "c_sh2")
            dma("sp", wr[:, :, :], w_router.rearrange("(kc p) n -> p kc n", p=128), [], [k_wr], "c_wr")
            dma("sp", brbc[:], bcast_row(b_router[0:1, :], NEXP), [], [k_brbc], "c_brbc")
            wupP_v = w_up_pool.rearrange("(kc p) n -> p kc n", p=128)
            wupA_v = w_up_attn.rearrange("(kc p) n -> p kc n", p=128)
            wout_v = w_out.rearrange("(kc p) n -> p kc n", p=128)
            ui = 0
            woi = 0
            for blk in range(4):
                tsl = slice(blk * 512, (blk + 1) * 512)
                dma("sp", hTb[:, :, :], hT_d[:, :, tsl], [("hT_d", blk)], [k_hTb], "c_hTb")
                dma("sp", ypb[:, :, :], ypT_d[:, :, tsl], [("ypT_d", blk)], [k_ypb], "c_ypb")
                dma("sp", oTb[:, :, :], oT_d[:, :, tsl], [("oT_d", blk)], [k_oTb], "c_oTb")
                for dc in range(16):
                    wu, k_wu = wup[ui % 2]
                    key = f"wup{ui % 2}"
                    ui += 1
                    dsl = slice(dc * 128, (dc + 1) * 128)
                    load_w(wu[:, 0:8, :], wupP_v[:, :, dsl], (k_wu, 0), key + "a")
                    load_w(wu[:, 8:24, :], wupA_v[:, :, dsl], (k_wu, 1), key + "b")
                    load_w(wu[:, 24:40, :], w_in_v[:, :, 5200 + dc * 128:5200 + (dc + 1) * 128], (k_wu, 2), key + "c")
                    load_w(wu[:, 40:56, :], w_in_v[:, :, 7248 + dc * 128:7248 + (dc + 1) * 128], (k_wu, 3), key + "d")
                    for kc in range(8):
                        mm(PS[0][:, :], wu[:, kc, :], ypb[:, kc, :], kc == 0, kc == 7, [(k_wu, 0), k_ypb], [psk(0)])
                    for kc in range(16):
                        mm(PS[1][:, :], wu[:, 8 + kc, :], oTb[:, kc, :], kc == 0, kc == 15, [(k_wu, 1), k_oTb], [psk(1)])
                    for kc in range(16):
                        mm(PS[2][:, :], wu[:, 24 + kc, :], hTb[:, kc, :], kc == 0, kc == 15, [(k_wu, 2), k_hTb], [psk(2)])
                    for kc in range(16):
                        mm(PS[3][:, :], wu[:, 40 + kc, :], hTb[:, kc, :], kc == 0, kc == 15, [(k_wu, 3), k_hTb], [psk(3)])
                    act(sg[0][0][:], PS[2][:, :], AF.Sigmoid, [psk(2)], [sg[0][1]])
                    act(sg[1][0][:], PS[3][:, :], AF.Sigmoid, [psk(3)], [sg[1][1]])
                    tt("dve", sg[2][0][:], PS[0][:, :], sg[0][0][:], ALU.mult, [psk(0), sg[0][1]], [sg[2][1]])
                    tt("dve", sg[3][0][:], PS[1][:, :], sg[1][0][:], ALU.mult, [psk(1), sg[1][1]], [sg[3][1]])
                    tt("pool", mrg[:, dc, :], sg[2][0][:], sg[3][0][:], ALU.add, [sg[2][1], sg[3][1]], [(k_mrg, dc)])
                mrg_keys = [(k_mrg, dc) for dc in range(16)]
                for ts_ in range(4):
                    t_ = blk * 4 + ts_
                    for db in range(4):
                        wo_, k_wo = wo[woi % 2]
                        key = f"wo{woi % 2}"
                        woi += 1
                        load_w(wo_[:, :, :], wout_v[:, :, db * 512:(db + 1) * 512], k_wo, key)
                        for kc in range(16):
                            mm(PS[4 + db][:, :], mrg[:, kc, ts_ * 128:(ts_ + 1) * 128], wo_[:, kc, :], kc == 0, kc == 15,
                               [k_wo, (k_mrg, kc)], [psk(4 + db)])
                    sq_, k_sq = ssq4[0]
                    ypk = [psk(4 + db) for db in range(4)]
                    for db in range(4):
                        act(junk4[:, db * 512:(db + 1) * 512], PS[4 + db][:, :], AF.Square, [psk(4 + db)],
                            [(k_junk4, db), (k_ssp, db)], accum=ssp[:, db:db + 1])
                    S.op("dve", lambda e, sq_=sq_: e.tensor_reduce(out=sq_[:], in_=ssp[:, 0:4], axis=AX.X, op=ALU.add),
                         [(k_ssp, db) for db in range(4)], [k_sq])
                    rstd_from_ssq(sq_[:], k_sq)
                    dma("sp", xt1[:], x[t_ * 128:(t_ + 1) * 128, :], [], [k_xt1], "c_xt1")
                    for db in range(4):
                        dsl = slice(db * 512, (db + 1) * 512)
                        stt(h2t[:, dsl], PS[4 + db][:, :], sq_[:, 0:1], gt1g[:, dsl], ALU.mult, ALU.mult,
                            [psk(4 + db), k_sq, k_gt1g], [(k_h2t, db)])
                    h2k = [(k_h2t, db) for db in range(4)]
                    tt("pool", xt1[:], xt1[:], h2t[:], ALU.add, [k_xt1] + h2k, [k_xt1])
                    dma("sp", x1_d[t_ * 128:(t_ + 1) * 128, :], xt1[:], [k_xt1], [("x1_d", t_)], "c_x1st")
                    sq2, k_sq2 = ssq4[1]
                    act(junk4[:], xt1[:], AF.Square, [k_xt1], [(k_junk4, db) for db in range(4)] + [k_sq2], accum=sq2[:])
                    rstd_from_ssq(sq2[:], k_sq2)
                    stt(h2t[:], xt1[:], sq2[:, 0:1], A2[:], ALU.mult, ALU.mult, [k_xt1, k_sq2, k_A2], h2k)
                    tt("pool", h2t[:], h2t[:], sh2[:], ALU.add, h2k + [k_sh2], h2k)
                    for kc in range(16):
                        bb = kc // 4
                        tr(PS[bb][:, (kc % 4) * 128:(kc % 4 + 1) * 128], h2t[:, kc * 128:(kc + 1) * 128], idf[:],
                           h2k + [k_idf], [psk(bb)])
                    for bb in range(4):
                        cp("act", h2Tf[:, bb * 4:(bb + 1) * 4, :], PS[bb][:, :].rearrange("p (a b) -> p a b", a=4),
                           [psk(bb)], [(k_h2Tf, bb)])
                        cp("dve", h2Tb[:, bb * 4:(bb + 1) * 4, :], PS[bb][:, :].rearrange("p (a b) -> p a b", a=4),
                           [psk(bb)], [(k_h2Tb, bb)])
                    dma("sp", h2T_d[:, :, t_ * 128:(t_ + 1) * 128], h2Tb[:, :, :], [(k_h2Tb, bb) for bb in range(4)],
                        [("h2T_d", t_)], "c_h2Tst")
                    for kc in range(16):
                        mm(PS[0][:, 0:NEXP], h2Tf[:, kc, :], wr[:, kc, :], kc == 0, kc == 15,
                           [(k_h2Tf, kc // 4), k_wr], [psk(0)])
                    tt("dve", lg[:], PS[0][:, 0:NEXP], brbc[:], ALU.add, [psk(0), k_brbc], [k_lg])
                    S.op("dve", lambda e: e.max(out=m8[:], in_=lg[:]), [k_lg], [k_m8])
                    ts("dve", sel[:], lg[:], m8[:, 3:4], None, ALU.is_ge, None, [k_lg, k_m8], [k_sel])
                    ts("dve", ssm[:, 0:1], m8[:, 0:1], -1.0, None, ALU.mult, None, [k_m8], [(k_ssm, 0)])
                    act(ee[:], lg[:], AF.Exp, [k_lg, (k_ssm, 0)], [k_ee], bias=ssm[:, 0:1])
                    tt("dve", ee[:], ee[:], sel[:], ALU.mult, [k_ee, k_sel], [k_ee])
                    S.op("dve", lambda e: e.tensor_reduce(out=ssm[:, 1:2], in_=ee[:], axis=AX.X, op=ALU.add), [k_ee], [(k_ssm, 1)])
                    S.op("dve", lambda e: e.reciprocal(out=ssm[:, 1:2], in_=ssm[:, 1:2]), [(k_ssm, 1)], [(k_ssm, 1)])
                    ts("dve", G_sb[:, t_, :], ee[:], ssm[:, 1:2], None, ALU.mult, None, [k_ee, (k_ssm, 1)], [(k_G, t_)])
            if dbg:
                dma("sp", G_d[:, :, :], G_sb[:, :, :], [(k_G, t_) for t_ in range(NT)], ["G_d"], "c_Gd")
            A.release(mC4)
            chk("C4")

            mD = A.mark()
            h2h, k_h2h = A.alloc("h2h", [128, 16, 1024], BF16)
            yacc, k_yacc = A.alloc("yacc", [128, 8, D], F32)
            actT, k_actT = A.alloc("actT", [128, 8, 1024], BF16)
            w1g = [A.alloc(f"w1g{i}", [128, 16, 256], BF16) for i in range(2)]
            w1l = [A.alloc(f"w1l{i}", [128, 16, 256], BF16) for i in range(2)]
            w2b = [A.alloc(f"w2b{i}", [128, 8, 512], BF16) for i in range(2)]
            b1s, k_b1s = A.alloc("b1s", [128, NEXP, 32], F32)
            b2s, k_b2s = A.alloc("b2s", [NEXP, D], F32)
            GT, k_GT = A.alloc("GT", [NEXP, 128], F32)
            epi_t, k_epi = A.alloc("epi", [128, 8, 512], F32)
            eg = [(epi_t[:, i, :], (k_epi, i)) for i in range(0, 2)]
            es_ = [(epi_t[:, 2 + i, :], (k_epi, 2 + i)) for i in range(0, 2)]
            el = [(epi_t[:, 4 + i, :], (k_epi, 4 + i)) for i in range(0, 2)]
            ea = [(epi_t[:, 6 + i, :], (k_epi, 6 + i)) for i in range(0, 2)]
            gt2g, k_gt2g = A.alloc("gt2g", [128, D], F32)
            ssqD, k_ssqD = A.alloc("ssqD", [128, 1], F32)
            dma("sp", b1s[:, :, :], b1T[:, :, :], [], [k_b1s], "c_b1s")
            dma("sp", b2s[:, :], b2[:, :], [], [k_b2s], "c_b2s")
            load_bc(gt2g, k_gt2g, 5, "c_gt2g")
            w1_v = w1.rearrange("e (kc p) n -> e p kc n", p=128)
            w2_v = w2.rearrange("e (kc p) n -> e p kc n", p=128)
            G_keys = [(k_G, t_) for t_ in range(NT)]
            wi1 = 0
            wi2 = 0
            epi = 0
            for half in range(2):
                for q4 in range(2):
                    b_ = half * 2 + q4
                    dma("sp", h2h[:, :, q4 * 512:(q4 + 1) * 512], h2T_d[:, :, b_ * 512:(b_ + 1) * 512],
                        [("h2T_d", t_) for t_ in range(b_ * 4, b_ * 4 + 4)], [(k_h2h, q4)], f"c_h2h{q4}")
                for tl in range(8):
                    t_ = half * 8 + tl
                    tr(PS[6][0:NEXP, 0:128], G_sb[:, t_, :], idf[:], [(k_G, t_), k_idf], [psk(6)])
                    cp("act", GT[:, :], PS[6][0:NEXP, 0:128], [psk(6)], [k_GT])
                    for db in range(4):
                        pb = 4 + db % 2
                        mm(PS[pb][:, :], GT[:, :], b2s[:, db * 512:(db + 1) * 512], True, True, [k_GT, k_b2s], [psk(pb)])
                        cp("dve", yacc[:, tl, db * 512:(db + 1) * 512], PS[pb][:, :], [psk(pb)], [(k_yacc, tl, db)])
                for e_ in range(NEXP):
                    for fh in range(2):
                        for fg in range(4):
                            fc0 = fh * 8 + fg * 2
                            wg_, k_wg = w1g[wi1 % 2]
                            wl_, k_wl = w1l[wi1 % 2]
                            kg, kl = f"w1g{wi1 % 2}", f"w1l{wi1 % 2}"
                            wi1 += 1
                            load_w(wg_[:, :, :], w1_v[e_][:, :, fc0 * 128:fc0 * 128 + 256], k_wg, kg)
                            load_w(wl_[:, :, :], w1_v[e_][:, :, D + fc0 * 128:D + fc0 * 128 + 256], k_wl, kl)
                            for fci in range(2):
                                fc = fc0 + fci
                                fl = fg * 2 + fci
                                for blk in range(2):
                                    pg = (epi % 2) * 2
                                    pl = pg + 1
                                    s_ = epi % 2
                                    epi += 1
                                    for kc in range(16):
                                        mm(PS[pg][:, :], wg_[:, kc, fci * 128:(fci + 1) * 128], h2h[:, kc, blk * 512:(blk + 1) * 512],
                                           kc == 0, kc == 15, [k_wg, (k_h2h, blk)], [psk(pg)])
                                    for kc in range(16):
                                        mm(PS[pl][:, :], wl_[:, kc, fci * 128:(fci + 1) * 128], h2h[:, kc, blk * 512:(blk + 1) * 512],
                                           kc == 0, kc == 15, [k_wl, (k_h2h, blk)], [psk(pl)])
                                    g_, k_g = eg[s_]
                                    sg_, k_sg = es_[s_]
                                    l_, k_l = el[s_]
                                    a_, k_a = ea[s_]
                                    ts("dve", g_, PS[pg][:, :], b1s[:, e_, fc:fc + 1], 7.0, ALU.add, ALU.min,
                                       [psk(pg), k_b1s], [k_g])
                                    act(sg_, g_, AF.Sigmoid, [k_g], [k_sg], scale=1.702)
                                    act(l_, PS[pl][:, :], AF.Identity, [psk(pl), k_b1s], [k_l], bias=b1s[:, e_, 16 + fc:17 + fc])
                                    ts("dve", l_, l_, -7.0, 7.0, ALU.max, ALU.min, [k_l], [k_l])
                                    tt("dve", a_, g_, sg_, ALU.mult, [k_g, k_sg], [k_a])
                                    stt(actT[:, fl, blk * 512:(blk + 1) * 512], l_, 1.0, a_, ALU.add, ALU.mult,
                                        [k_l, k_a], [(k_actT, fl, blk)])
                        for db in range(4):
                            w2_, k_w2 = w2b[wi2 % 2]
                            k2 = f"w2b{wi2 % 2}"
                            wi2 += 1
                            load_w(w2_[:, :, :], w2_v[e_][:, fh * 8:(fh + 1) * 8, db * 512:(db + 1) * 512], k_w2, k2)
                            for tl in range(8):
                                t_ = half * 8 + tl
                                pb = 4 + tl % 4
                                for fl in range(8):
                                    mm(PS[pb][:, :], actT[:, fl, tl * 128:(tl + 1) * 128], w2_[:, fl, :], fl == 0, fl == 7,
                                       [(k_actT, fl, tl // 4), k_w2], [psk(pb)])
                                stt(yacc[:, tl, db * 512:(db + 1) * 512], PS[pb][:, :], G_sb[:, t_, e_:e_ + 1],
                                    yacc[:, tl, db * 512:(db + 1) * 512], ALU.mult, ALU.add,
                                    [psk(pb), (k_G, t_), (k_yacc, tl, db)], [(k_yacc, tl, db)])
                for tl in range(8):
                    t_ = half * 8 + tl
                    yk = [(k_yacc, tl, db) for db in range(4)]
                    jk = [(k_actT, fl_, b__) for fl_ in range(2) for b__ in range(2)]
                    x1t = epi_t[:, 0:4, :].rearrange("p a b -> p (a b)")
                    k_x1l = [(k_epi, i_) for i_ in range(4)]
                    act(actT[:, 0:2, :].rearrange("p a b -> p (a b)"), yacc[:, tl, :], AF.Square, yk, jk + [k_ssqD], accum=ssqD[:])
                    rstd_from_ssq(ssqD[:], k_ssqD)
                    dma("sp", x1t, x1_d[t_ * 128:(t_ + 1) * 128, :], [("x1_d", t_)], k_x1l, "c_x1t")
                    stt(yacc[:, tl, :], yacc[:, tl, :], ssqD[:, 0:1], gt2g[:], ALU.mult, ALU.mult, yk + [k_ssqD, k_gt2g], yk)
                    tt("pool", x1t, x1t, yacc[:, tl, :], ALU.add, k_x1l + yk, k_x1l)
                    dma("sp", out[t_ * 128:(t_ + 1) * 128, :], x1t, k_x1l, [("out", t_)], "c_out")
            A.release(mD)

        except _Stop:
            pass
        S.emit(nc, st, final_waits=list(S.dma_counts.keys()))
    return nc


def _consts():
    t = np.arange(T, dtype=np.float32)

    def tabs(rot_dim, head_dim):
        half = rot_dim // 2
        inv = (np.float32(500000.0) ** (-np.arange(0, rot_dim, 2, dtype=np.float32) / np.float32(rot_dim))).astype(np.float32)
        ang = (t[:, None] * inv[None, :]).astype(np.float32)
        cos = np.cos(ang).astype(np.float32).T
        sin = np.sin(ang).astype(np.float32).T
        C = np.ones((128, T), np.float32)
        Sg = np.zeros((128, T), np.float32)
        Pm = np.zeros((128, 128), np.float32)
        for hb in range(0, 128, head_dim):
            C[hb:hb + half] = cos
            C[hb + half:hb + rot_dim] = cos
            Sg[hb:hb + half] = -sin
            Sg[hb + half:hb + rot_dim] = sin
            for i in range(half):
                Pm[hb + half + i, hb + i] = 1.0
                Pm[hb + i, hb + half + i] = 1.0
        return np.stack([C, Sg]), Pm

    rA, pA = tabs(32, 128)
    rI, pI = tabs(16, 64)
    tri = np.where(np.arange(128)[None, :] <= np.arange(128)[:, None], 0.0, -1.0e4).astype(np.float32)
    pinv = np.zeros((128, 4, 16), np.float32)
    for g, w in enumerate((2, 4, 8, 16)):
        pinv[:, g, :] = 1.0 / np.minimum(np.arange(16) + 1, w).astype(np.float32)
    return dict(identf=np.eye(128, dtype=np.float32), ropeA=rA, ropeI=rI,
                perms=np.stack([pA, pI]).astype(np.float32), trineg=tri, poolinv=pinv)


def make_in_maps(x, c, w_ada, b_ada, g_pre_mix, g_post_mix, w_in, w_pool_grp, pool_scale, w_up_pool, w_up_attn,
                 w_out, g_pre_ffn, g_post_ffn, w_router, b_router, w1, b1, w2, b2):
    f = lambda a: np.ascontiguousarray(np.asarray(a, dtype=np.float32))
    x = f(x)
    c = f(c)
    shared = dict(
        w_ada=f(w_ada)[0], b_ada=f(b_ada)[0][None, :],
        gvec=np.ascontiguousarray(np.stack([f(g_pre_mix)[0], f(g_post_mix)[0], f(g_pre_ffn)[0], f(g_post_ffn)[0]])),
        w_in=f(w_in)[0], w_pool=f(w_pool_grp)[0],
        pscale=np.ascontiguousarray(f(pool_scale)[0].reshape(8, 128).T),
        w_up_pool=f(w_up_pool)[0], w_up_attn=f(w_up_attn)[0], w_out=f(w_out)[0],
        w_router=f(w_router)[0], b_router=f(b_router)[0][None, :],
        w1=f(w1)[0], b1T=np.ascontiguousarray(f(b1)[0].reshape(NEXP, 32, 128).transpose(2, 0, 1)),
        w2=f(w2)[0], b2=f(b2)[0],
    )
    shared.update(_consts())
    maps = []
    for b in range(8):
        m = dict(shared)
        m["x"] = np.ascontiguousarray(x[b])
        m["cT"] = np.ascontiguousarray(c[b].reshape(16, 128).T)
        maps.append(m)
    return maps


def kernel(**inputs):
    in_maps = make_in_maps(**inputs)
    nc = build(dbg=False)
    res = run_bass_kernel_spmd(nc, in_maps, core_ids=list(range(8)))
    return np.stack([np.asarray(r["out"], dtype=np.float32) for r in res.results], axis=0)
```

```python
import numpy as np
from contextlib import ExitStack
import concourse.bass as bass
import concourse.mybir as mybir
from concourse.bass_utils import run_bass_kernel_spmd

F32 = mybir.dt.float32
BF16 = mybir.dt.bfloat16
AF = mybir.ActivationFunctionType
ALU = mybir.AluOpType
AX = mybir.AxisListType

D = 2048
T = 2048
NT = 16
NEXP = 32
EPS = 1e-6
ENGS = ("pe", "act", "dve", "pool", "sp")
CAP = 30000
SB_BASE = 16512
SB_END = 229376


class Op:
    __slots__ = ("eng", "fn", "reads", "writes", "dma_key", "ordinal", "waits", "signals",
                 "sigidx", "dma_cnt", "clock", "dma_clock", "alias")


def kname(k):
    return k[0] if isinstance(k, tuple) else k


class Sched:
    def __init__(self):
        self.ops = []
        self.per_eng = {e: [] for e in ENGS}
        self.dma_counts = {}

    def op(self, eng, fn, reads=(), writes=(), dma_key=None):
        o = Op()
        o.eng = eng
        o.fn = fn
        def _flat(xs):
            r = []
            for x_ in xs:
                if isinstance(x_, list):
                    r.extend(_flat(x_))
                else:
                    r.append(x_)
            return tuple(r)
        o.reads = _flat(reads)
        o.writes = _flat(writes)
        o.dma_key = dma_key
        o.signals = False
        o.sigidx = 0
        o.waits = []
        o.alias = None
        o.dma_cnt = 0
        o.ordinal = len(self.per_eng[eng])
        if dma_key is not None:
            self.dma_counts[dma_key] = self.dma_counts.get(dma_key, 0) + 1
            o.dma_cnt = self.dma_counts[dma_key]
        self.per_eng[eng].append(o)
        self.ops.append(o)
        return o

    def alias(self, new_name, old_names):
        o = Op()
        o.eng = None
        o.alias = (new_name, tuple(old_names))
        self.ops.append(o)

    def resolve(self):
        last_writer = {}
        readers = {}
        by_name = {}
        inherit = {}
        clock = {e: {} for e in ENGS}
        dclock = {e: {} for e in ENGS}

        def touch(k):
            if k not in readers:
                n = kname(k)
                readers[k] = list(inherit.get(n, ()))
                by_name.setdefault(n, []).append(k)

        for o in self.ops:
            if o.eng is None:
                new, olds = o.alias
                lst = inherit.setdefault(new, [])
                for on in olds:
                    lst.extend(inherit.get(on, ()))
                    for k in by_name.get(on, ()):
                        w = last_writer.get(k)
                        if w is not None:
                            lst.append(w)
                        lst.extend(readers.get(k, ()))
                best = {}
                for d in lst:
                    kk = ("d", d.dma_key) if d.dma_key is not None else ("e", d.eng)
                    val = d.dma_cnt if d.dma_key is not None else d.ordinal
                    if kk not in best or best[kk][0] < val:
                        best[kk] = (val, d)
                inherit[new] = [v[1] for v in best.values()]
                continue
            deps = []
            for r in o.reads:
                touch(r)
                w = last_writer.get(r)
                if w is not None:
                    deps.append(w)
                if kname(r) == "ps":
                    for k2 in by_name.get("ps", ()):
                        if k2[1] == r[1]:
                            deps.extend(o2 for o2 in readers[k2] if o2.eng != o.eng)
            for wk in o.writes:
                touch(wk)
                w = last_writer.get(wk)
                if w is not None:
                    deps.append(w)
                deps.extend(readers[wk])
            ck = clock[o.eng]
            dk = dclock[o.eng]
            need_e = {}
            need_d = {}
            for d in deps:
                if d is o:
                    continue
                if d.dma_key is not None:
                    if dk.get(d.dma_key, 0) >= d.dma_cnt:
                        continue
                    if need_d.get(d.dma_key, (0, None))[0] < d.dma_cnt:
                        need_d[d.dma_key] = (d.dma_cnt, d)
                else:
                    if d.eng == o.eng and o.eng == "pe" and o.dma_key is None:
                        continue
                    if ck.get(d.eng, -1) >= d.ordinal:
                        continue
                    if need_e.get(d.eng, (-1, None))[0] < d.ordinal:
                        need_e[d.eng] = (d.ordinal, d)
            waits = []
            for e, (ordn, d) in need_e.items():
                d.signals = True
                waits.append(("e", d))
                ck[e] = max(ck.get(e, -1), ordn)
                for e2, v in d.clock.items():
                    if e2 != o.eng and ck.get(e2, -1) < v:
                        ck[e2] = v
                for k2, v in d.dma_clock.items():
                    if dk.get(k2, 0) < v:
                        dk[k2] = v
            for k, (cnt, d) in need_d.items():
                waits.append(("d", d))
                dk[k] = max(dk.get(k, 0), cnt)
                for e2, v in d.clock.items():
                    if e2 != o.eng and ck.get(e2, -1) < v:
                        ck[e2] = v
                for k2, v in d.dma_clock.items():
                    if dk.get(k2, 0) < v:
                        dk[k2] = v
            o.waits = waits
            o.clock = dict(ck)
            o.dma_clock = dict(dk)
            for r in o.reads:
                readers[r].append(o)
            for wk in o.writes:
                last_writer[wk] = o
                readers[wk] = []
        self.nsig = {}
        for e in ENGS:
            n = 0
            for o in self.per_eng[e]:
                if o.dma_key is None and o.signals:
                    n += 1
                    o.sigidx = n
            self.nsig[e] = n

    def emit(self, nc, stack, final_waits=()):
        self.resolve()
        esems = {}
        for e in ENGS:
            nep = (self.nsig[e] + CAP - 1) // CAP
            esems[e] = [stack.enter_context(nc.semaphore(f"s_{e}_{i}")) for i in range(nep)]
        dsems = {}
        for i, k in enumerate(self.dma_counts):
            dsems[k] = stack.enter_context(nc.semaphore(f"d_{i}"))
        block = stack.enter_context(nc.Block())

        def run(e, engobj):
            for o in self.per_eng[e]:
                for kind, d in o.waits:
                    if kind == "e":
                        ep, val = (d.sigidx - 1) // CAP, (d.sigidx - 1) % CAP + 1
                        engobj.wait_ge(esems[d.eng][ep], val)
                    else:
                        engobj.wait_ge(dsems[d.dma_key], 16 * d.dma_cnt)
                ins = o.fn(engobj)
                if o.dma_key is not None:
                    ins.then_inc(dsems[o.dma_key], 16)
                elif o.signals:
                    ep = (o.sigidx - 1) // CAP
                    ins.then_inc(esems[e][ep], 1)
            if e == "sp":
                for k in final_waits:
                    engobj.wait_ge(dsems[k], 16 * self.dma_counts[k])

        @block.tensor
        def _(eng):
            run("pe", eng)

        @block.scalar
        def _(eng):
            run("act", eng)

        @block.vector
        def _(eng):
            run("dve", eng)

        @block.gpsimd
        def _(eng):
            run("pool", eng)

        @block.sync
        def _(eng):
            run("sp", eng)


class Arena:
    def __init__(self, nc, S):
        self.nc = nc
        self.S = S
        self.top = SB_BASE
        self.live = []
        self.freed = []
        self.cnt = 0

    def alloc(self, name, shape, dtype):
        esz = 4 if dtype == F32 else 2
        n = 1
        for s in shape[1:]:
            n *= s
        nbytes = (n * esz + 63) // 64 * 64
        start = self.top
        end = start + nbytes
        assert end <= SB_END, f"SBUF overflow allocating {name}: {end}"
        self.cnt += 1
        uname = f"{name}_{self.cnt}"
        h = self.nc.alloc_sbuf_tensor_at(uname, list(shape), dtype, offset=start)
        olds = [f[2] for f in self.freed if f[0] < end and start < f[1]]
        if olds:
            self.S.alias(uname, olds)
        self.live.append((start, end, uname))
        self.top = end
        return h, uname

    def mark(self):
        return (self.top, len(self.live))

    def release(self, m):
        top, n = m
        for rec in self.live[n:]:
            self.freed.append(rec)
        del self.live[n:]
        self.top = top


class _Stop(Exception):
    pass


def build(dbg=False, stop=None):
    nc = bass.Bass("TRN2", target_bir_lowering=False)
    S = Sched()

    def din(name, shape, dt=F32):
        return nc.dram_tensor(name, list(shape), dt, kind="ExternalInput").ap()

    x = din("x", [T, D])
    cT = din("cT", [128, 16])
    w_ada = din("w_ada", [D, 6 * D])
    b_ada = din("b_ada", [1, 6 * D])
    gvec = din("gvec", [4, D])
    w_in = din("w_in", [D, 9296])
    w_pool = din("w_pool", [4, 256, 256])
    pscale = din("pscale", [128, 8])
    w_up_pool = din("w_up_pool", [1024, D])
    w_up_attn = din("w_up_attn", [D, D])
    w_out = din("w_out", [D, D])
    w_router = din("w_router", [D, NEXP])
    b_router = din("b_router", [1, NEXP])
    w1 = din("w1", [NEXP, D, 2 * D] if stop is None else [1, 128, 128])
    b1T = din("b1T", [128, NEXP, 32])
    w2 = din("w2", [NEXP, D, D] if stop is None else [1, 128, 128])
    b2 = din("b2", [NEXP, D])
    identf = din("identf", [128, 128])
    ropeA = din("ropeA", [2, 128, T])
    ropeI = din("ropeI", [2, 128, T])
    perms = din("perms", [2, 128, 128])
    trineg = din("trineg", [128, 128])
    poolinv = din("poolinv", [128, 4, 16])

    kind_s = "ExternalOutput" if dbg else "Internal"
    out = nc.dram_tensor("out", [T, D], F32, kind="ExternalOutput").ap()
    modrow = nc.dram_tensor("modrow", [6, D], F32, kind=kind_s).ap()
    hT_d = nc.dram_tensor("hT_d", [128, 16, T], BF16, kind=kind_s).ap()
    ypT_d = nc.dram_tensor("ypT_d", [128, 8, T], BF16, kind=kind_s).ap()
    oT_d = nc.dram_tensor("oT_d", [128, 16, T], BF16, kind=kind_s).ap()
    x1_d = nc.dram_tensor("x1_d", [T, D], F32, kind=kind_s).ap()
    h2T_d = nc.dram_tensor("h2T_d", [128, 16, T], BF16, kind=kind_s).ap()
    G_d = nc.dram_tensor("G_d", [128, NT, NEXP], F32, kind=kind_s).ap()
    mk_d = nc.dram_tensor("mk_d", [T, T], BF16, kind=kind_s).ap() if dbg else None

    with ExitStack() as st:
        A = Arena(nc, S)
        PS = [st.enter_context(nc.psum_tensor(f"psb{i}", [128, 512], F32)) for i in range(8)]
        PSB = [p.bitcast(BF16) for p in PS]

        def psk(b, sub=None):
            return ("ps", b) if sub is None else ("ps", b, sub)

        def dma(eng, out_ap, in_ap, reads, writes, key):
            S.op(eng, lambda e: e.dma_start(out=out_ap, in_=in_ap), reads, writes, dma_key=key)

        def mm(out_ap, lhsT, rhs, start, stop, reads, writes):
            S.op("pe", lambda e: e.matmul(out_ap, lhsT=lhsT, rhs=rhs, start=start, stop=stop), reads, writes)

        def tr(out_ap, in_ap, ident, reads, writes):
            S.op("pe", lambda e: e.transpose(out=out_ap, in_=in_ap, identity=ident), reads, writes)

        def act(out_ap, in_ap, func, reads, writes, bias=None, scale=None, accum=None):
            kw = {}
            if bias is not None:
                kw["bias"] = bias
            if scale is not None:
                kw["scale"] = scale
            if accum is not None:
                kw["accum_out"] = accum
            S.op("act", lambda e: e.activation(out=out_ap, in_=in_ap, func=func, **kw), reads, writes)

        def ts(eng, out_ap, in0, s1, s2, op0, op1, reads, writes, accum=None):
            if op1 is None:
                S.op(eng, lambda e: e.tensor_scalar(out=out_ap, in0=in0, scalar1=s1, scalar2=None, op0=op0), reads, writes)
            elif accum is None:
                S.op(eng, lambda e: e.tensor_scalar(out=out_ap, in0=in0, scalar1=s1, scalar2=s2, op0=op0, op1=op1), reads, writes)
            else:
                S.op(eng, lambda e: e.tensor_scalar(out=out_ap, in0=in0, scalar1=s1, scalar2=s2, op0=op0, op1=op1, accum_out=accum), reads, writes)

        def tt(eng, out_ap, in0, in1, op, reads, writes):
            S.op(eng, lambda e: e.tensor_tensor(out=out_ap, in0=in0, in1=in1, op=op), reads, writes)

        def stt(out_ap, in0, scalar, in1, op0, op1, reads, writes):
            S.op("dve", lambda e: e.scalar_tensor_tensor(out=out_ap, in0=in0, scalar=scalar, in1=in1, op0=op0, op1=op1), reads, writes)

        def cp(eng, out_ap, in_ap, reads, writes):
            if eng == "act":
                S.op("act", lambda e: e.copy(out=out_ap, in_=in_ap), reads, writes)
            else:
                S.op(eng, lambda e: e.tensor_copy(out=out_ap, in_=in_ap), reads, writes)

        def bcast_row(dram_ap_row, n):
            return bass.AP(tensor=dram_ap_row.tensor, offset=dram_ap_row.offset, ap=[[0, 128], [1, n]])

        def rstd_from_ssq(ssq, nm):
            ts("dve", ssq, ssq, 1.0 / D, EPS, ALU.mult, ALU.add, [nm], [nm])
            act(ssq, ssq, AF.Sqrt, [nm], [nm])
            S.op("dve", lambda e: e.reciprocal(out=ssq, in_=ssq), [nm], [nm])

        idf, k_idf = A.alloc("identf", [128, 128], F32)
        idb, k_idb = A.alloc("identb", [128, 128], BF16)
        dma("sp", idf[:], identf[:, :], [], [k_idf], "c_idf")
        dma("pool", idb[:], identf[:, :], [], [k_idb], "c_idb")
        G_sb, k_G = A.alloc("G", [128, NT, NEXP], F32)

        w_in_v = w_in.rearrange("(kc p) n -> p kc n", p=128)

        def chk(name):
            if stop == name:
                raise _Stop()

        try:
            mA = A.mark()
            cact, k_cact = A.alloc("cact", [128, 16], F32)
            crep, k_crep = A.alloc("crep", [128, 16, 128], F32)
            modbc = []
            for j in range(6):
                modbc.append(A.alloc(f"modbc{j}", [128, D], F32))
            gbc, k_gbc = A.alloc("gbc", [128, D], F32)
            babc, k_babc = A.alloc("babc", [128, D], F32)
            wa = [A.alloc(f"wa{i}", [128, 16, 512], F32) for i in range(2)]
            dma("sp", cact[:], cT[:, :], [], [k_cact], "c_cact")
            act(cact[:], cact[:], AF.Silu, [k_cact], [k_cact])
            for kc in range(16):
                cp("dve", crep[:, kc, :], cact[:, kc:kc + 1].to_broadcast([128, 128]), [k_cact], [(k_crep, kc)])
            w_ada_v = w_ada.rearrange("(kc p) n -> p kc n", p=128)
            it = 0
            for j in range(6):
                dma("sp", babc[:], bcast_row(b_ada[0:1, j * D:(j + 1) * D], D), [], [k_babc], "c_babc")
                for cb in range(4):
                    slot = it % 2
                    wt, k_wt = wa[slot]
                    c0 = j * D + cb * 512
                    dma("sp", wt[:, :, :], w_ada_v[:, :, c0:c0 + 512], [], [k_wt], f"wa{slot}")
                    pb = it % 2
                    for kc in range(16):
                        mm(PS[pb][:, :], crep[:, kc, :], wt[:, kc, :], kc == 0, kc == 15,
                           [(k_crep, kc), k_wt], [psk(pb)])
                    tt("dve", modbc[j][0][:, cb * 512:(cb + 1) * 512], PS[pb][:, :], babc[:, cb * 512:(cb + 1) * 512],
                       ALU.add, [psk(pb), k_babc], [(modbc[j][1], cb)])
                    it += 1
            def allk(j):
                return [(modbc[j][1], cb) for cb in range(4)]

            def derive(jmod, grow, add_one, outrow):
                dma("sp", gbc[:], bcast_row(gvec[grow:grow + 1, :], D), [], [k_gbc], "c_gbc")
                if add_one:
                    stt(modbc[jmod][0][:, :], modbc[jmod][0][:, :], 1.0, gbc[:, :], ALU.add, ALU.mult,
                        allk(jmod) + [k_gbc], allk(jmod))
                else:
                    tt("dve", modbc[jmod][0][:, :], modbc[jmod][0][:, :], gbc[:, :], ALU.mult,
                       allk(jmod) + [k_gbc], allk(jmod))
                dma("sp", modrow[outrow:outrow + 1, :], modbc[jmod][0][0:1, :], allk(jmod), [("modrow", outrow)], f"modrow{outrow}")

            derive(1, 0, True, 0)
            dma("sp", modrow[1:2, :], modbc[0][0][0:1, :], allk(0), [("modrow", 1)], "modrow1")
            derive(2, 1, False, 2)
            derive(4, 2, True, 3)
            dma("sp", modrow[4:5, :], modbc[3][0][0:1, :], allk(3), [("modrow", 4)], "modrow4")
            derive(5, 3, False, 5)
            A.release(mA)
            chk("A")

            def load_bc(dst, k_dst, row, key):
                dma("sp", dst[:], bcast_row(modrow[row:row + 1, :], D), [("modrow", row)], [k_dst], key)

            mB = A.mark()
            hT, k_hT = A.alloc("hT", [128, 16, T], BF16)
            mB2 = A.mark()
            A1, k_A1 = A.alloc("A1", [128, D], F32)
            sh1, k_sh1 = A.alloc("sh1", [128, D], F32)
            load_bc(A1, k_A1, 0, "c_A1")
            load_bc(sh1, k_sh1, 1, "c_sh1")
            xt = [A.alloc(f"xt{i}", [128, D], F32) for i in range(2)]
            hb = [A.alloc(f"hb{i}", [128, D], BF16) for i in range(2)]
            junk, k_junk = A.alloc("junkB", [128, D], BF16)
            ssq = [A.alloc(f"ssq{i}", [128, 1], F32) for i in range(2)]

            def norm_mod_tile(src, k_src, dst, k_dst, Abc, k_Abc, shbc, k_shbc, ssq_t, k_ssq, junk_t, k_junk_t):
                act(junk_t[:], src[:], AF.Square, [k_src], [k_junk_t, k_ssq], accum=ssq_t[:])
                rstd_from_ssq(ssq_t[:], k_ssq)
                stt(src[:], src[:], ssq_t[:, 0:1], Abc[:], ALU.mult, ALU.mult, [k_src, k_ssq, k_Abc], [k_src])
                tt("pool", dst[:], src[:], shbc[:], ALU.add, [k_src, k_shbc], [k_dst])

            for t_ in range(NT):
                s_ = t_ % 2
                xt_, k_xt = xt[s_]
                hb_, k_hb = hb[s_]
                dma("sp", xt_[:], x[t_ * 128:(t_ + 1) * 128, :], [], [k_xt], f"xt{s_}")
                norm_mod_tile(xt_, k_xt, hb_, k_hb, A1, k_A1, sh1, k_sh1, ssq[s_][0], ssq[s_][1], junk, k_junk)
                b0 = 2 * s_
                for kc in range(16):
                    bb = b0 + kc // 8
                    tr(PSB[bb][:, (kc % 8) * 128:(kc % 8 + 1) * 128], hb_[:, kc * 128:(kc + 1) * 128], idb[:],
                       [k_hb, k_idb], [psk(bb)])
                for hh in range(2):
                    cp("act" if hh == 0 else "dve", hT[:, hh * 8:(hh + 1) * 8, t_ * 128:(t_ + 1) * 128],
                       PSB[b0 + hh][:, :].rearrange("p (a b) -> p a b", a=8), [psk(b0 + hh)], [(k_hT, t_)])
            A.release(mB2)
            for hh in range(4):
                dma("sp", hT_d[:, :, hh * 512:(hh + 1) * 512], hT[:, :, hh * 512:(hh + 1) * 512],
                    [(k_hT, t_) for t_ in range(hh * 4, hh * 4 + 4)], [("hT_d", hh)], f"hT_d{hh}")
            hT_keys = [(k_hT, t_) for t_ in range(NT)]
            chk("B")

            def hT_blk_keys(blk):
                return [(k_hT, t_) for t_ in range(blk * 4, blk * 4 + 4)]

            def load_w(dst_ap, src_ap, k_dst, key):
                dma("pool", dst_ap, src_ap, [], [k_dst], key)

            mC3 = A.mark()
            wb = [A.alloc(f"wbP{i}", [128, 16, 512], BF16) for i in range(2)]
            wgrp, k_wgrp = A.alloc("wgrp", [128, 4, 2, 256], BF16)
            psc, k_psc = A.alloc("psc", [128, 8], F32)
            pinv, k_pinv = A.alloc("pinv", [128, 4, 16], F32)
            ub = [A.alloc(f"ub{i}", [128, 16 + T], F32) for i in range(3)]
            mixT, k_mixT = A.alloc("mixT", [128, 8, T], BF16)
            t16, k_t16 = A.alloc("t16", [128, 16], F32)
            ypst = [A.alloc(f"ypst{i}", [128, 512], BF16) for i in range(2)]
            for g in range(4):
                load_w(wgrp[:, g, :, :], w_pool[g].rearrange("(cc p) d -> p cc d", p=128), (k_wgrp, g), f"c_wgrp{g}")
            dma("sp", psc[:], pscale[:, :], [], [k_psc], "c_psc")
            dma("sp", pinv[:, :, :], poolinv[:, :, :], [], [k_pinv], "c_pinv")
            for i in range(3):
                S.op("pool", lambda e, i=i: e.memset(ub[i][0][:, 0:16], 0.0), [], [(ub[i][1], "pad")])
            pbi = 0
            for grp in range(2):
                wt, k_wt = wb[grp % 2]
                load_w(wt[:, :, :], w_in_v[:, :, grp * 512:(grp + 1) * 512], k_wt, f"wbP{grp % 2}")
                for oc in range(4):
                    c = grp * 4 + oc
                    g = c // 2
                    u_, k_u = ub[0]
                    for blk in range(4):
                        pb = pbi % 2
                        pbi += 1
                        for kc in range(16):
                            mm(PS[pb][:, :], wt[:, kc, oc * 128:(oc + 1) * 128], hT[:, kc, blk * 512:(blk + 1) * 512],
                               kc == 0, kc == 15, [k_wt] + hT_blk_keys(blk), [psk(pb)])
                        cp("act", u_[:, 16 + blk * 512:16 + (blk + 1) * 512], PS[pb][:, :], [psk(pb)], [(k_u, blk)])
                    ukeys = [(k_u, b_) for b_ in range(4)] + [(k_u, "pad")]
                    cur, k_cur = u_, ukeys
                    dst_i = 1
                    d_ = 1
                    for step in range(g + 1):
                        nxt, k_nx = ub[dst_i]
                        tt("dve", nxt[:, 16:16 + T], cur[:, 16:16 + T], cur[:, 16 - d_:16 - d_ + T], ALU.add,
                           k_cur, [(k_nx, "all")])
                        cur, k_cur = nxt, [(k_nx, "all"), (k_nx, "pad")]
                        dst_i = 3 - dst_i
                        d_ *= 2
                    w_ = 2 ** (g + 1)
                    stt(mixT[:, c, :], cur[:, 16:16 + T], 1.0 / w_, u_[:, 16:16 + T], ALU.mult, ALU.subtract,
                        k_cur + ukeys, [(k_mixT, c)])
                    tt("dve", t16[:], cur[:, 16:32], pinv[:, g, :], ALU.mult, k_cur + [k_pinv], [k_t16])
                    tt("dve", mixT[:, c, 0:16], t16[:], u_[:, 16:32], ALU.subtract, [k_t16] + ukeys, [(k_mixT, c)])
            si = 0
            for g in range(4):
                for dd in range(2):
                    oc_ = g * 2 + dd
                    for blk in range(4):
                        pb = pbi % 2
                        pbi += 1
                        for cc in range(2):
                            mm(PS[pb][:, :], wgrp[:, g, cc, dd * 128:(dd + 1) * 128], mixT[:, g * 2 + cc, blk * 512:(blk + 1) * 512],
                               cc == 0, cc == 1, [(k_wgrp, g), (k_mixT, g * 2 + cc)], [psk(pb)])
                        ys, k_ys = ypst[si % 2]
                        si += 1
                        ts("dve", ys[:], PS[pb][:, :], psc[:, oc_:oc_ + 1], None, ALU.mult, None, [psk(pb), k_psc], [k_ys])
                        dma("sp", ypT_d[:, oc_, blk * 512:(blk + 1) * 512], ys[:], [k_ys], [("ypT_d", blk)], f"ypst{si % 2}")
            A.release(mC3)
            chk("C3")

            def rope(pb, rb, perm, k_perm, tab, k_tab, blk, raw_t, tmp1_t, tmp2_t, dst_ap, dst_keys, psl=slice(0, 128)):
                raw, k_raw = raw_t
                t1, k_t1 = tmp1_t
                t2, k_t2 = tmp2_t
                RM = 3
                if RM != 11:
                    cp("act", raw[:], PS[pb][:, :], [psk(pb)], [k_raw])
                if RM == 12:
                    cp("dve", t1[:], PS[pb][:, :], [psk(pb)], [k_t1])
                    return
                if RM == 11:
                    tt("dve", t1[:], PS[pb][:, :], tab[:, 0, blk * 512:(blk + 1) * 512], ALU.mult, [psk(pb), k_tab], [k_t1])
                    return
                if RM >= 2:
                    mm(PS[rb][:, :], perm, raw[:], True, True, [k_perm, k_raw], [psk(rb)])
                if RM == 0:
                    return
                tt("dve", t1[:], PS[pb][:, :], tab[:, 0, blk * 512:(blk + 1) * 512], ALU.mult, [psk(pb), k_tab], [k_t1])
                if RM == 10:
                    return
                if RM >= 3:
                    tt("dve", t2[:], PS[rb][:, :], tab[:, 1, blk * 512:(blk + 1) * 512], ALU.mult, [psk(rb), k_tab], [k_t2])
                    tt("dve", dst_ap, t1[psl, :], t2[psl, :], ALU.add, [k_t1, k_t2], dst_keys)
                else:
                    cp("pool", dst_ap, t1[psl, :], [k_t1], dst_keys)

            mC1 = A.mark()
            MOFF = []
            off = 0
            for j in range(NT):
                MOFF.append(off)
                off += T - 128 * j
            maskT, k_maskT = A.alloc("maskT", [128, off], BF16)
            mC1b = A.mark()
            qiT, k_qiT = A.alloc("qiT", [128, 8, T], BF16)
            kiT, k_kiT = A.alloc("kiT", [128, T], BF16)
            wi, k_wi = A.alloc("wi", [128, NT, 16], F32)
            wabs, k_wabs = A.alloc("wabs", [128, NT, 16], F32)
            wsgn, k_wsgn = A.alloc("wsgn", [128, NT, 16], F32)
            mC1c = A.mark()
            wb = [A.alloc(f"wbI{i}", [128, 16, 512], BF16) for i in range(2)]
            wkiA, k_wkiA = A.alloc("wkiA", [128, 16, 128], BF16)
            wkiB, k_wkiB = A.alloc("wkiB", [128, 16, 128], BF16)
            rtab, k_rtab = A.alloc("rtabI", [128, 2, T], F32)
            permI, k_permI = A.alloc("permI", [128, 128], BF16)
            raws = [A.alloc(f"rawI{i}", [128, 512], BF16) for i in range(2)]
            t1s = [A.alloc(f"t1I{i}", [128, 512], F32) for i in range(2)]
            t2s = [A.alloc(f"t2I{i}", [128, 512], F32) for i in range(2)]
            dma("sp", rtab[:, 0, :], ropeI[0], [], [(k_rtab, 0)], "c_rtabI0")
            dma("sp", rtab[:, 1, :], ropeI[1], [], [(k_rtab, 1)], "c_rtabI1")
            k_rt = [(k_rtab, 0), (k_rtab, 1)]
            dma("pool", permI[:], perms[1], [], [k_permI], "c_permI")
            load_w(wkiA[:, :, :], w_in_v[:, :, 5056:5184], k_wkiA, "c_wki0")
            load_w(wkiB[:, :, :], w_in_v[:, :, 5120:5248], k_wkiB, "c_wki1")
            ri = 0
            chk("C1a0")
            for grp in range(2):
                if grp == 1:
                    chk("C1a1")
                wt, k_wt = wb[grp % 2]
                load_w(wt[:, :, :], w_in_v[:, :, 4096 + grp * 512:4096 + (grp + 1) * 512], k_wt, f"wbI{grp % 2}")
                for oc in range(4):
                    c = grp * 4 + oc
                    for blk in range(4):
                        pb = ri % 2
                        for kc in range(16):
                            mm(PS[pb][:, :], wt[:, kc, oc * 128:(oc + 1) * 128], hT[:, kc, blk * 512:(blk + 1) * 512],
                               kc == 0, kc == 15, [k_wt] + hT_blk_keys(blk), [psk(pb)])
                        rope(pb, 2 + pb, permI[:], k_permI, rtab, k_rt[0:2], blk, raws[pb], t1s[pb], t2s[pb],
                             qiT[:, c, blk * 512:(blk + 1) * 512], [(k_qiT, c, blk)])
                        ri += 1
            for blk in range(4):
                for (wk__, k_wk__, psl_, hf_) in ((wkiA, k_wkiA, slice(64, 128), 1), (wkiB, k_wkiB, slice(0, 64), 0)):
                    pb = ri % 2
                    for kc in range(16):
                        mm(PS[pb][:, :], wk__[:, kc, :], hT[:, kc, blk * 512:(blk + 1) * 512], kc == 0, kc == 15,
                           [k_wk__] + hT_blk_keys(blk), [psk(pb)])
                    rope(pb, 2 + pb, permI[:], k_permI, rtab, k_rt, blk, raws[pb], t1s[pb], t2s[pb],
                         kiT[psl_, blk * 512:(blk + 1) * 512], [(k_kiT, blk, hf_)], psl=psl_)
                    ri += 1
            chk("C1a2")
            for t_ in range(NT):
                pb = 4 + t_ % 2
                for kc in range(16):
                    mm(PS[pb][:, 0:16], hT[:, kc, t_ * 128:(t_ + 1) * 128], wkiB[:, kc, 64:80], kc == 0, kc == 15,
                       [k_wkiB, (k_hT, t_)], [psk(pb)])
                ts("dve", wi[:, t_, :], PS[pb][:, 0:16], 1.0 / 32.0, None, ALU.mult, None, [psk(pb)], [(k_wi, t_)])
            wi_keys = [(k_wi, t_) for t_ in range(NT)]
            chk("C1a3")
            act(wsgn[:, :, :], wi[:, :, :], AF.Sign, wi_keys, [k_wsgn])
            tt("dve", wabs[:, :, :], wi[:, :, :], wsgn[:, :, :], ALU.mult, wi_keys + [k_wsgn], [k_wabs])
            A.release(mC1c)
            chk("C1a")
            sc = [A.alloc(f"sc{i}", [128, T], F32) for i in range(2)]
            rr = [A.alloc(f"rr{i}", [128, 512], F32) for i in range(4)]
            mk = [A.alloc(f"mk{i}", [128, T], BF16) for i in range(2)]
            jnk, k_jnk = A.alloc("jnkI", [128, T], BF16)
            tneg, k_tneg = A.alloc("tneg", [128, 128], F32)
            sm = [A.alloc(f"sm{i}", [128, 8], F32) for i in range(2)]
            dma("sp", tneg[:], trineg[:, :], [], [k_tneg], "c_tneg")
            NIT = 18
            rri = 0
            qi_keys_c = lambda c, i: [(k_qiT, c, i // 4)]
            for i in range(NT):
                L = 128 * (i + 1)
                nsb = (L + 511) // 512
                s0, k_s0 = sc[0]
                s1, k_s1 = sc[1]
                for h in range(16):
                    c, half = h // 2, h % 2
                    base = 64 * half
                    acc, k_acc = (s0, k_s0) if half == 0 else (s1, k_s1)
                    for sb in range(nsb):
                        n = min(512, L - 512 * sb)
                        pb = rri % 4
                        r_, k_r = rr[rri % 4]
                        rri += 1
                        mm(PS[pb][:, 0:n], qiT[base:base + 64, c, i * 128:(i + 1) * 128], kiT[base:base + 64, sb * 512:sb * 512 + n],
                           True, True, [(k_qiT, c, i // 4), (k_kiT, sb, half)], [psk(pb)])
                        act(r_[:, 0:n], PS[pb][:, 0:n], AF.Relu, [psk(pb), k_wabs], [k_r], scale=wabs[:, i, h:h + 1])
                        if h < 2:
                            ts("dve", acc[:, sb * 512:sb * 512 + n], r_[:, 0:n], wsgn[:, i, h:h + 1], None, ALU.mult, None,
                               [k_r, k_wsgn], [(k_acc, sb)])
                        else:
                            stt(acc[:, sb * 512:sb * 512 + n], r_[:, 0:n], wsgn[:, i, h:h + 1], acc[:, sb * 512:sb * 512 + n],
                                ALU.mult, ALU.add, [k_r, k_wsgn, (k_acc, sb)], [(k_acc, sb)])
                sk0 = [(k_s0, sb) for sb in range(nsb)]
                sk1 = [(k_s1, sb) for sb in range(nsb)]
                tt("pool", s0[:, 0:L], s0[:, 0:L], s1[:, 0:L], ALU.add, sk0 + sk1, sk0)
                sm_, k_sm = sm[i % 2]
                mk_, k_mk = mk[i % 2]
                if i >= 2:
                    S.op("dve", lambda e, s0=s0, sm_=sm_, L=L: e.tensor_reduce(out=sm_[:, 5:6], in_=s0[:, 0:L], axis=AX.X, op=ALU.max),
                         sk0, [(k_sm, 5)])
                    S.op("dve", lambda e, s0=s0, sm_=sm_, L=L: e.tensor_reduce(out=sm_[:, 0:1], in_=s0[:, 0:L], axis=AX.X, op=ALU.min),
                         sk0, [(k_sm, 0)])
                    tt("dve", sm_[:, 1:2], sm_[:, 5:6], sm_[:, 0:1], ALU.subtract, [(k_sm, 5), (k_sm, 0)], [(k_sm, 1)])
                tt("dve", s0[:, L - 128:L], s0[:, L - 128:L], tneg[:], ALU.add, sk0 + [k_tneg], sk0)
                if i >= 2:
                    for it_ in range(NIT):
                        f = 2.0 ** (-(it_ + 1))
                        stt(sm_[:, 2:3], sm_[:, 1:2], f, sm_[:, 0:1], ALU.mult, ALU.add, [(k_sm, 1), (k_sm, 0)], [(k_sm, 2)])
                        ts("dve", jnk[:, 0:L], s0[:, 0:L], sm_[:, 2:3], 0.0, ALU.is_ge, ALU.add, sk0 + [(k_sm, 2)],
                           [k_jnk, (k_sm, 3)], accum=sm_[:, 3:4])
                        ts("dve", sm_[:, 4:5], sm_[:, 3:4], 255.5, f, ALU.is_ge, ALU.mult, [(k_sm, 3)], [(k_sm, 4)])
                        stt(sm_[:, 0:1], sm_[:, 4:5], sm_[:, 1:2], sm_[:, 0:1], ALU.mult, ALU.add,
                            [(k_sm, 4), (k_sm, 1), (k_sm, 0)], [(k_sm, 0)])
                    ts("dve", mk_[:, 0:L], s0[:, 0:L], sm_[:, 0:1], None, ALU.is_ge, None, sk0 + [(k_sm, 0)], [k_mk])
                else:
                    ts("dve", mk_[:, 0:L], s0[:, 0:L], -1.0e3, None, ALU.is_ge, None, sk0, [k_mk])
                if dbg:
                    dma("sp", mk_d[i * 128:(i + 1) * 128, 0:L], mk_[:, 0:L], [k_mk], [("mk_d", i)], f"mkd{i % 2}")
                for j in range(i + 1):
                    bb = 4 + (j // 8) + 2 * (i % 2)
                    tr(PSB[bb][:, (j % 8) * 128:(j % 8 + 1) * 128], mk_[:, j * 128:(j + 1) * 128], idb[:], [k_mk, k_idb], [psk(bb)])
                for j in range(i + 1):
                    bb = 4 + (j // 8) + 2 * (i % 2)
                    o_ = MOFF[j] + (i - j) * 128
                    cp("act" if j % 2 == 0 else "dve", maskT[:, o_:o_ + 128], PSB[bb][:, (j % 8) * 128:(j % 8 + 1) * 128],
                       [psk(bb)], [(k_maskT, j, i)])
            A.release(mC1b)
            chk("C1")

            mC2 = A.mark()
            wb = [A.alloc(f"wbA{i}", [128, 16, 512], BF16) for i in range(2)]
            wkv = [A.alloc(f"wkv{i}", [128, 16, 256], BF16) for i in range(1)]
            rtabA, k_rtabA = A.alloc("rtabA", [128, 2, T], F32)
            permA, k_permA = A.alloc("permA", [128, 128], BF16)
            raws = [A.alloc(f"rawA{i}", [128, 512], BF16) for i in range(2)]
            t1s = [A.alloc(f"t1A{i}", [128, 512], F32) for i in range(2)]
            t2s = [A.alloc(f"t2A{i}", [128, 512], F32) for i in range(2)]
            qT = [A.alloc(f"qT{i}", [128, 4, T], BF16) for i in range(1)]
            kT_, k_kT = A.alloc("kT", [128, T], BF16)
            vg, k_vg = A.alloc("vg", [128, NT, 130], BF16)
            ex = [A.alloc(f"ex{i}", [128, 512], BF16) for i in range(3)]
            pp = [A.alloc(f"pp{i}", [128, 512], BF16) for i in range(3)]
            rden = [A.alloc(f"rden{i}", [128, 1], F32) for i in range(2)]
            otm = [A.alloc(f"otm{i}", [128, 128], BF16) for i in range(2)]
            ost = [A.alloc(f"ost{i}", [128, 512], BF16) for i in range(2)]
            dma("sp", rtabA[:, 0, :], ropeA[0], [], [(k_rtabA, 0)], "c_rtabA0")
            dma("sp", rtabA[:, 1, :], ropeA[1], [], [(k_rtabA, 1)], "c_rtabA1")
            k_rtA = [(k_rtabA, 0), (k_rtabA, 1)]
            dma("pool", permA[:], perms[0], [], [k_permA], "c_permA")
            SCL = 128.0 ** -0.5
            ri = 0
            exi = 0
            osti = 0
            for g in range(4):
                wt, k_wt = wb[g % 2]
                wk_, k_wk = wkv[0]
                load_w(wt[:, :, :], w_in_v[:, :, 1024 + g * 512:1024 + (g + 1) * 512], k_wt, f"wbA{g % 2}")
                load_w(wk_[:, :, 0:128], w_in_v[:, :, 3072 + g * 128:3072 + (g + 1) * 128], (k_wk, 0), "wkv_a")
                load_w(wk_[:, :, 128:256], w_in_v[:, :, 3584 + g * 128:3584 + (g + 1) * 128], (k_wk, 1), "wkv_b")
                q_, k_q = qT[0]
                for r in range(4):
                    for blk in range(4):
                        pb = ri % 2
                        for kc in range(16):
                            mm(PS[pb][:, :], wt[:, kc, r * 128:(r + 1) * 128], hT[:, kc, blk * 512:(blk + 1) * 512],
                               kc == 0, kc == 15, [k_wt] + hT_blk_keys(blk), [psk(pb)])
                        rope(pb, 2 + pb, permA[:], k_permA, rtabA, k_rtA, blk, raws[pb], t1s[pb], t2s[pb],
                             q_[:, r, blk * 512:(blk + 1) * 512], [(k_q, r, blk)])
                        ri += 1
                for blk in range(4):
                    pb = ri % 2
                    for kc in range(16):
                        mm(PS[pb][:, :], wk_[:, kc, 0:128], hT[:, kc, blk * 512:(blk + 1) * 512], kc == 0, kc == 15,
                           [(k_wk, 0)] + hT_blk_keys(blk), [psk(pb)])
                    rope(pb, 2 + pb, permA[:], k_permA, rtabA, k_rtA, blk, raws[pb], t1s[pb], t2s[pb],
                         kT_[:, blk * 512:(blk + 1) * 512], [(k_kT, blk)])
                    ri += 1
                S.op("pool", lambda e: e.memset(vg[:, :, 128:130], 1.0), [], [(k_vg, "one")])
                for t_ in range(NT):
                    pb = ri % 2
                    ri += 1
                    for kc in range(16):
                        mm(PS[pb][:, 0:128], hT[:, kc, t_ * 128:(t_ + 1) * 128], wk_[:, kc, 128:256], kc == 0, kc == 15,
                           [(k_wk, 1), (k_hT, t_)], [psk(pb)])
                    cp("act", vg[:, t_, 0:128], PS[pb][:, 0:128], [psk(pb)], [(k_vg, t_)])
                for r in range(4):
                    h = 4 * g + r
                    for qb in range(4):
                        nj = 4 * qb + 4
                        for j in range(nj):
                            t_lo = max(qb * 512, j * 128)
                            n = qb * 512 + 512 - t_lo
                            pb = exi % 2
                            e_, k_e = ex[exi % 3]
                            p_, k_p = pp[exi % 3]
                            exi += 1
                            mm(PS[pb][:, 0:n], kT_[:, j * 128:(j + 1) * 128], q_[:, r, t_lo:t_lo + n], True, True,
                               [(k_kT, j // 4), (k_q, r, qb)], [psk(pb)])
                            act(e_[:, 0:n], PS[pb][:, 0:n], AF.Exp, [psk(pb)], [k_e], scale=SCL)
                            mo = MOFF[j] + (t_lo - 128 * j)
                            mkeys = [(k_maskT, j, i_) for i_ in range(t_lo // 128, t_lo // 128 + n // 128)]
                            tt("dve", p_[:, 0:n], e_[:, 0:n], maskT[:, mo:mo + n], ALU.mult,
                               [k_e] + mkeys, [k_p])
                            for ts_ in range((t_lo - qb * 512) // 128, 4):
                                col = (qb * 512 + ts_ * 128) - t_lo
                                ab = 4 + ts_
                                ac = 0
                                jlast = 4 * qb + ts_
                                mm(PS[ab][:, ac:ac + 129], p_[:, col:col + 128], vg[:, j, 0:129], j == 0, j == jlast,
                                   [k_p, (k_vg, j), (k_vg, "one")], [psk(ab)])
                        os_, k_os = ost[osti % 2]
                        osti += 1
                        for ts_ in range(4):
                            ab = 4 + ts_
                            ac = 0
                            rd, k_rd = rden[ts_ % 2]
                            om, k_om = otm[ts_ % 2]
                            S.op("dve", lambda e, rd=rd, ab=ab, ac=ac: e.reciprocal(out=rd[:], in_=PS[ab][:, ac + 128:ac + 129]),
                                 [psk(ab)], [k_rd])
                            ts("dve", om[:], PS[ab][:, ac:ac + 128], rd[:, 0:1], None, ALU.mult, None,
                               [psk(ab), k_rd], [k_om])
                            tb = 2 + ts_ % 2
                            tr(PSB[tb][:, 0:128], om[:], idb[:], [k_om, k_idb], [psk(tb)])
                            cp("act", os_[:, ts_ * 128:(ts_ + 1) * 128], PSB[tb][:, 0:128], [psk(tb)], [(k_os, ts_)])
                        dma("sp", oT_d[:, h, qb * 512:(qb + 1) * 512], os_[:], [(k_os, ts_) for ts_ in range(4)],
                            [("oT_d", qb)], f"ost{osti % 2}")
            A.release(mC2)
            A.release(mC1)
            A.release(mB)
            chk("C2")

            mC4 = A.mark()
            wup = [A.alloc(f"wup{i}", [128, 56, 128], BF16) for i in range(2)]
            hTb, k_hTb = A.alloc("hTb", [128, 16, 512], BF16)
            ypb, k_ypb = A.alloc("ypb", [128, 8, 512], BF16)
            oTb, k_oTb = A.alloc("oTb", [128, 16, 512], BF16)
            mrg, k_mrg = A.alloc("mrg", [128, 16, 512], BF16)
            wo = [A.alloc(f"wo{i}", [128, 16, 512], BF16) for i in range(2)]
            sg = [A.alloc(f"sg{i}", [128, 512], F32) for i in range(4)]
            xt1, k_xt1 = A.alloc("xt1", [128, D], F32)
            h2t, k_h2t = A.alloc("h2t", [128, D], F32)
            gt1g, k_gt1g = A.alloc("gt1g", [128, D], F32)
            A2, k_A2 = A.alloc("A2", [128, D], F32)
            sh2, k_sh2 = A.alloc("sh2", [128, D], F32)
            h2Tf, k_h2Tf = A.alloc("h2Tf", [128, 16, 128], F32)
            h2Tb, k_h2Tb = A.alloc("h2Tb", [128, 16, 128], BF16)
            junk4, k_junk4 = A.alloc("junk4", [128, D], BF16)
            ssq4 = [A.alloc(f"ssq4{i}", [128, 1], F32) for i in range(2)]
            ssp, k_ssp = A.alloc("ssp", [128, 4], F32)
            wr, k_wr = A.alloc("wr", [128, 16, NEXP], F32)
            brbc, k_brbc = A.alloc("brbc", [128, NEXP], F32)
            lg, k_lg = A.alloc("lg", [128, NEXP], F32)
            m8, k_m8 = A.alloc("m8", [128, 8], F32)
            sel, k_sel = A.alloc("sel", [128, NEXP], F32)
            ee, k_ee = A.alloc("ee", [128, NEXP], F32)
            ssm, k_ssm = A.alloc("ssm", [128, 2], F32)
            load_bc(gt1g, k_gt1g, 2, "c_gt1g")
            load_bc(A2, k_A2, 3, "c_A2")
            load_bc(sh2, k_sh2, 4, "c_sh2")
            dma("sp", wr[:, :, :], w_router.rearrange("(kc p) n -> p kc n", p=128), [], [k_wr], "c_wr")
            dma("sp", brbc[:], bcast_row(b_router[0:1, :], NEXP), [], [k_brbc], "c_brbc")
            wupP_v = w_up_pool.rearrange("(kc p) n -> p kc n", p=128)
            wupA_v = w_up_attn.rearrange("(kc p) n -> p kc n", p=128)
            wout_v = w_out.rearrange("(kc p) n -> p kc n", p=128)
            ui = 0
            woi = 0
            for blk in range(4):
                tsl = slice(blk * 512, (blk + 1) * 512)
                dma("sp", hTb[:, :, :], hT_d[:, :, tsl], [("hT_d", blk)], [k_hTb], "c_hTb")
                dma("sp", ypb[:, :, :], ypT_d[:, :, tsl], [("ypT_d", blk)], [k_ypb], "c_ypb")
                dma("sp", oTb[:, :, :], oT_d[:, :, tsl], [("oT_d", blk)], [k_oTb], "c_oTb")
                for dc in range(16):
                    wu, k_wu = wup[ui % 2]
                    key = f"wup{ui % 2}"
                    ui += 1
                    dsl = slice(dc * 128, (dc + 1) * 128)
                    load_w(wu[:, 0:8, :], wupP_v[:, :, dsl], (k_wu, 0), key + "a")
                    load_w(wu[:, 8:24, :], wupA_v[:, :, dsl], (k_wu, 1), key + "b")
                    load_w(wu[:, 24:40, :], w_in_v[:, :, 5200 + dc * 128:5200 + (dc + 1) * 128], (k_wu, 2), key + "c")
                    load_w(wu[:, 40:56, :], w_in_v[:, :, 7248 + dc * 128:7248 + (dc + 1) * 128], (k_wu, 3), key + "d")
                    for kc in range(8):
                        mm(PS[0][:, :], wu[:, kc, :], ypb[:, kc, :], kc == 0, kc == 7, [(k_wu, 0), k_ypb], [psk(0)])
                    for kc in range(16):
                        mm(PS[1][:, :], wu[:, 8 + kc, :], oTb[:, kc, :], kc == 0, kc == 15, [(k_wu, 1), k_oTb], [psk(1)])
                    for kc in range(16):
                        mm(PS[2][:, :], wu[:, 24 + kc, :], hTb[:, kc, :], kc == 0, kc == 15, [(k_wu, 2), k_hTb], [psk(2)])
                    for kc in range(16):
                        mm(PS[3][:, :], wu[:, 40 + kc, :], hTb[:, kc, :], kc == 0, kc == 15, [(k_wu, 3), k_hTb], [psk(3)])
                    act(sg[0][0][:], PS[2][:, :], AF.Sigmoid, [psk(2)], [sg[0][1]])
                    act(sg[1][0][:], PS[3][:, :], AF.Sigmoid, [psk(3)], [sg[1][1]])
                    tt("dve", sg[2][0][:], PS[0][:, :], sg[0][0][:], ALU.mult, [psk(0), sg[0][1]], [sg[2][1]])
                    tt("dve", sg[3][0][:], PS[1][:, :], sg[1][0][:], ALU.mult, [psk(1), sg[1][1]], [sg[3][1]])
                    tt("dve", mrg[:, dc, :], sg[2][0][:], sg[3][0][:], ALU.add, [sg[2][1], sg[3][1]], [(k_mrg, dc)])
                mrg_keys = [(k_mrg, dc) for dc in range(16)]
                for ts_ in range(4):
                    t_ = blk * 4 + ts_
                    for db in range(4):
                        wo_, k_wo = wo[woi % 2]
                        key = f"wo{woi % 2}"
                        woi += 1
                        load_w(wo_[:, :, :], wout_v[:, :, db * 512:(db + 1) * 512], k_wo, key)
                        for kc in range(16):
                            mm(PS[4 + db][:, :], mrg[:, kc, ts_ * 128:(ts_ + 1) * 128], wo_[:, kc, :], kc == 0, kc == 15,
                               [k_wo, (k_mrg, kc)], [psk(4 + db)])
                    sq_, k_sq = ssq4[0]
                    ypk = [psk(4 + db) for db in range(4)]
                    for db in range(4):
                        act(junk4[:, db * 512:(db + 1) * 512], PS[4 + db][:, :], AF.Square, [psk(4 + db)],
                            [(k_junk4, db), (k_ssp, db)], accum=ssp[:, db:db + 1])
                    S.op("dve", lambda e, sq_=sq_: e.tensor_reduce(out=sq_[:], in_=ssp[:, 0:4], axis=AX.X, op=ALU.add),
                         [(k_ssp, db) for db in range(4)], [k_sq])
                    rstd_from_ssq(sq_[:], k_sq)
                    dma("sp", xt1[:], x[t_ * 128:(t_ + 1) * 128, :], [], [k_xt1], "c_xt1")
                    for db in range(4):
                        dsl = slice(db * 512, (db + 1) * 512)
                        stt(h2t[:, dsl], PS[4 + db][:, :], sq_[:, 0:1], gt1g[:, dsl], ALU.mult, ALU.mult,
                            [psk(4 + db), k_sq, k_gt1g], [(k_h2t, db)])
                    h2k = [(k_h2t, db) for db in range(4)]
                    tt("pool", xt1[:], xt1[:], h2t[:], ALU.add, [k_xt1] + h2k, [k_xt1])
                    dma("sp", x1_d[t_ * 128:(t_ + 1) * 128, :], xt1[:], [k_xt1], [("x1_d", t_)], "c_x1st")
                    sq2, k_sq2 = ssq4[1]
                    act(junk4[:], xt1[:], AF.Square, [k_xt1], [(k_junk4, db) for db in range(4)] + [k_sq2], accum=sq2[:])
                    rstd_from_ssq(sq2[:], k_sq2)
                    stt(h2t[:], xt1[:], sq2[:, 0:1], A2[:], ALU.mult, ALU.mult, [k_xt1, k_sq2, k_A2], h2k)
                    tt("pool", h2t[:], h2t[:], sh2[:], ALU.add, h2k + [k_sh2], h2k)
                    for kc in range(16):
                        bb = kc // 4
                        tr(PS[bb][:, (kc % 4) * 128:(kc % 4 + 1) * 128], h2t[:, kc * 128:(kc + 1) * 128], idf[:],
                           h2k + [k_idf], [psk(bb)])
                    for bb in range(4):
                        cp("act", h2Tf[:, bb * 4:(bb + 1) * 4, :], PS[bb][:, :].rearrange("p (a b) -> p a b", a=4),
                           [psk(bb)], [(k_h2Tf, bb)])
                        cp("dve", h2Tb[:, bb * 4:(bb + 1) * 4, :], PS[bb][:, :].rearrange("p (a b) -> p a b", a=4),
                           [psk(bb)], [(k_h2Tb, bb)])
                    dma("sp", h2T_d[:, :, t_ * 128:(t_ + 1) * 128], h2Tb[:, :, :], [(k_h2Tb, bb) for bb in range(4)],
                        [("h2T_d", t_)], "c_h2Tst")
                    for kc in range(16):
                        mm(PS[0][:, 0:NEXP], h2Tf[:, kc, :], wr[:, kc, :], kc == 0, kc == 15,
                           [(k_h2Tf, kc // 4), k_wr], [psk(0)])
                    tt("dve", lg[:], PS[0][:, 0:NEXP], brbc[:], ALU.add, [psk(0), k_brbc], [k_lg])
                    S.op("dve", lambda e: e.max(out=m8[:], in_=lg[:]), [k_lg], [k_m8])
                    ts("dve", sel[:], lg[:], m8[:, 3:4], None, ALU.is_ge, None, [k_lg, k_m8], [k_sel])
                    ts("dve", ssm[:, 0:1], m8[:, 0:1], -1.0, None, ALU.mult, None, [k_m8], [(k_ssm, 0)])
                    act(ee[:], lg[:], AF.Exp, [k_lg, (k_ssm, 0)], [k_ee], bias=ssm[:, 0:1])
                    tt("dve", ee[:], ee[:], sel[:], ALU.mult, [k_ee, k_sel], [k_ee])
                    S.op("dve", lambda e: e.tensor_reduce(out=ssm[:, 1:2], in_=ee[:], axis=AX.X, op=ALU.add), [k_ee], [(k_ssm, 1)])
                    S.op("dve", lambda e: e.reciprocal(out=ssm[:, 1:2], in_=ssm[:, 1:2]), [(k_ssm, 1)], [(k_ssm, 1)])
                    ts("dve", G_sb[:, t_, :], ee[:], ssm[:, 1:2], None, ALU.mult, None, [k_ee, (k_ssm, 1)], [(k_G, t_)])
            if dbg:
                dma("sp", G_d[:, :, :], G_sb[:, :, :], [(k_G, t_) for t_ in range(NT)], ["G_d"], "c_Gd")
            A.release(mC4)
            chk("C4")

            mD = A.mark()
            h2h, k_h2h = A.alloc("h2h", [128, 16, 1024], BF16)
            yacc, k_yacc = A.alloc("yacc", [128, 8, D], F32)
            actT, k_actT = A.alloc("actT", [128, 8, 1024], BF16)
            w1g = [A.alloc(f"w1g{i}", [128, 16, 256], BF16) for i in range(2)]
            w1l = [A.alloc(f"w1l{i}", [128, 16, 256], BF16) for i in range(2)]
            w2b = [A.alloc(f"w2b{i}", [128, 8, 512], BF16) for i in range(2)]
            b1s, k_b1s = A.alloc("b1s", [128, NEXP, 32], F32)
            b2s, k_b2s = A.alloc("b2s", [NEXP, D], F32)
            GT, k_GT = A.alloc("GT", [NEXP, 128], F32)
            epi_t, k_epi = A.alloc("epi", [128, 8, 512], F32)
            eg = [(epi_t[:, i, :], (k_epi, i)) for i in range(0, 2)]
            es_ = [(epi_t[:, 2 + i, :], (k_epi, 2 + i)) for i in range(0, 2)]
            el = [(epi_t[:, 4 + i, :], (k_epi, 4 + i)) for i in range(0, 2)]
            ea = [(epi_t[:, 6 + i, :], (k_epi, 6 + i)) for i in range(0, 2)]
            gt2g, k_gt2g = A.alloc("gt2g", [128, D], F32)
            ssqD, k_ssqD = A.alloc("ssqD", [128, 1], F32)
            dma("sp", b1s[:, :, :], b1T[:, :, :], [], [k_b1s], "c_b1s")
            dma("sp", b2s[:, :], b2[:, :], [], [k_b2s], "c_b2s")
            load_bc(gt2g, k_gt2g, 5, "c_gt2g")
            w1_v = w1.rearrange("e (kc p) n -> e p kc n", p=128)
            w2_v = w2.rearrange("e (kc p) n -> e p kc n", p=128)
            G_keys = [(k_G, t_) for t_ in range(NT)]
            wi1 = 0
            wi2 = 0
            epi = 0
            for half in range(2):
                for q4 in range(2):
                    b_ = half * 2 + q4
                    dma("sp", h2h[:, :, q4 * 512:(q4 + 1) * 512], h2T_d[:, :, b_ * 512:(b_ + 1) * 512],
                        [("h2T_d", t_) for t_ in range(b_ * 4, b_ * 4 + 4)], [(k_h2h, q4)], f"c_h2h{q4}")
                for tl in range(8):
                    t_ = half * 8 + tl
                    tr(PS[6][0:NEXP, 0:128], G_sb[:, t_, :], idf[:], [(k_G, t_), k_idf], [psk(6)])
                    cp("act", GT[:, :], PS[6][0:NEXP, 0:128], [psk(6)], [k_GT])
                    for db in range(4):
                        pb = 4 + db % 2
                        mm(PS[pb][:, :], GT[:, :], b2s[:, db * 512:(db + 1) * 512], True, True, [k_GT, k_b2s], [psk(pb)])
                        cp("dve", yacc[:, tl, db * 512:(db + 1) * 512], PS[pb][:, :], [psk(pb)], [(k_yacc, tl, db)])
                for e_ in range(NEXP):
                    for fh in range(2):
                        for fg in range(4):
                            fc0 = fh * 8 + fg * 2
                            wg_, k_wg = w1g[wi1 % 2]
                            wl_, k_wl = w1l[wi1 % 2]
                            kg, kl = f"w1g{wi1 % 2}", f"w1l{wi1 % 2}"
                            wi1 += 1
                            load_w(wg_[:, :, :], w1_v[e_][:, :, fc0 * 128:fc0 * 128 + 256], k_wg, kg)
                            load_w(wl_[:, :, :], w1_v[e_][:, :, D + fc0 * 128:D + fc0 * 128 + 256], k_wl, kl)
                            for fci in range(2):
                                fc = fc0 + fci
                                fl = fg * 2 + fci
                                for blk in range(2):
                                    pg = (epi % 2) * 2
                                    pl = pg + 1
                                    s_ = epi % 2
                                    epi += 1
                                    for kc in range(16):
                                        mm(PS[pg][:, :], wg_[:, kc, fci * 128:(fci + 1) * 128], h2h[:, kc, blk * 512:(blk + 1) * 512],
                                           kc == 0, kc == 15, [k_wg, (k_h2h, blk)], [psk(pg)])
                                    for kc in range(16):
                                        mm(PS[pl][:, :], wl_[:, kc, fci * 128:(fci + 1) * 128], h2h[:, kc, blk * 512:(blk + 1) * 512],
                                           kc == 0, kc == 15, [k_wl, (k_h2h, blk)], [psk(pl)])
                                    g_, k_g = eg[s_]
                                    sg_, k_sg = es_[s_]
                                    l_, k_l = el[s_]
                                    a_, k_a = ea[s_]
                                    ts("dve", g_, PS[pg][:, :], b1s[:, e_, fc:fc + 1], 7.0, ALU.add, ALU.min,
                                       [psk(pg), k_b1s], [k_g])
                                    act(sg_, g_, AF.Sigmoid, [k_g], [k_sg], scale=1.702)
                                    act(l_, PS[pl][:, :], AF.Identity, [psk(pl), k_b1s], [k_l], bias=b1s[:, e_, 16 + fc:17 + fc])
                                    ts("dve", l_, l_, -7.0, 7.0, ALU.max, ALU.min, [k_l], [k_l])
                                    tt("dve", a_, g_, sg_, ALU.mult, [k_g, k_sg], [k_a])
                                    stt(actT[:, fl, blk * 512:(blk + 1) * 512], l_, 1.0, a_, ALU.add, ALU.mult,
                                        [k_l, k_a], [(k_actT, fl, blk)])
                        for db in range(4):
                            w2_, k_w2 = w2b[wi2 % 2]
                            k2 = f"w2b{wi2 % 2}"
                            wi2 += 1
                            load_w(w2_[:, :, :], w2_v[e_][:, fh * 8:(fh + 1) * 8, db * 512:(db + 1) * 512], k_w2, k2)
                            for tl in range(8):
                                t_ = half * 8 + tl
                                pb = 4 + tl % 4
                                for fl in range(8):
                                    mm(PS[pb][:, :], actT[:, fl, tl * 128:(tl + 1) * 128], w2_[:, fl, :], fl == 0, fl == 7,
                                       [(k_actT, fl, tl // 4), k_w2], [psk(pb)])
                                stt(yacc[:, tl, db * 512:(db + 1) * 512], PS[pb][:, :], G_sb[:, t_, e_:e_ + 1],
                                    yacc[:, tl, db * 512:(db + 1) * 512], ALU.mult, ALU.add,
                                    [psk(pb), (k_G, t_), (k_yacc, tl, db)], [(k_yacc, tl, db)])
                for tl in range(8):
                    t_ = half * 8 + tl
                    yk = [(k_yacc, tl, db) for db in range(4)]
                    jk = [(k_actT, fl_, b__) for fl_ in range(2) for b__ in range(2)]
                    x1t = epi_t[:, 0:4, :].rearrange("p a b -> p (a b)")
                    k_x1l = [(k_epi, i_) for i_ in range(4)]
                    act(actT[:, 0:2, :].rearrange("p a b -> p (a b)"), yacc[:, tl, :], AF.Square, yk, jk + [k_ssqD], accum=ssqD[:])
                    rstd_from_ssq(ssqD[:], k_ssqD)
                    dma("sp", x1t, x1_d[t_ * 128:(t_ + 1) * 128, :], [("x1_d", t_)], k_x1l, "c_x1t")
                    stt(yacc[:, tl, :], yacc[:, tl, :], ssqD[:, 0:1], gt2g[:], ALU.mult, ALU.mult, yk + [k_ssqD, k_gt2g], yk)
                    tt("pool", x1t, x1t, yacc[:, tl, :], ALU.add, k_x1l + yk, k_x1l)
                    dma("sp", out[t_ * 128:(t_ + 1) * 128, :], x1t, k_x1l, [("out", t_)], "c_out")
            A.release(mD)

        except _Stop:
            pass
        S.emit(nc, st, final_waits=list(S.dma_counts.keys()))
    return nc


def _consts():
    t = np.arange(T, dtype=np.float32)

    def tabs(rot_dim, head_dim):
        half = rot_dim // 2
        inv = (np.float32(500000.0) ** (-np.arange(0, rot_dim, 2, dtype=np.float32) / np.float32(rot_dim))).astype(np.float32)
        ang = (t[:, None] * inv[None, :]).astype(np.float32)
        cos = np.cos(ang).astype(np.float32).T
        sin = np.sin(ang).astype(np.float32).T
        C = np.ones((128, T), np.float32)
        Sg = np.zeros((128, T), np.float32)
        Pm = np.zeros((128, 128), np.float32)
        for hb in range(0, 128, head_dim):
            C[hb:hb + half] = cos
            C[hb + half:hb + rot_dim] = cos
            Sg[hb:hb + half] = -sin
            Sg[hb + half:hb + rot_dim] = sin
            for i in range(half):
                Pm[hb + half + i, hb + i] = 1.0
                Pm[hb + i, hb + half + i] = 1.0
        return np.stack([C, Sg]), Pm

    rA, pA = tabs(32, 128)
    rI, pI = tabs(16, 64)
    tri = np.where(np.arange(128)[None, :] <= np.arange(128)[:, None], 0.0, -1.0e4).astype(np.float32)
    pinv = np.zeros((128, 4, 16), np.float32)
    for g, w in enumerate((2, 4, 8, 16)):
        pinv[:, g, :] = 1.0 / np.minimum(np.arange(16) + 1, w).astype(np.float32)
    return dict(identf=np.eye(128, dtype=np.float32), ropeA=rA, ropeI=rI,
                perms=np.stack([pA, pI]).astype(np.float32), trineg=tri, poolinv=pinv)


def make_in_maps(x, c, w_ada, b_ada, g_pre_mix, g_post_mix, w_in, w_pool_grp, pool_scale, w_up_pool, w_up_attn,
                 w_out, g_pre_ffn, g_post_ffn, w_router, b_router, w1, b1, w2, b2):
    f = lambda a: np.ascontiguousarray(np.asarray(a, dtype=np.float32))
    x = f(x)
    c = f(c)
    shared = dict(
        w_ada=f(w_ada)[0], b_ada=f(b_ada)[0][None, :],
        gvec=np.ascontiguousarray(np.stack([f(g_pre_mix)[0], f(g_post_mix)[0], f(g_pre_ffn)[0], f(g_post_ffn)[0]])),
        w_in=f(w_in)[0], w_pool=f(w_pool_grp)[0],
        pscale=np.ascontiguousarray(f(pool_scale)[0].reshape(8, 128).T),
        w_up_pool=f(w_up_pool)[0], w_up_attn=f(w_up_attn)[0], w_out=f(w_out)[0],
        w_router=f(w_router)[0], b_router=f(b_router)[0][None, :],
        w1=f(w1)[0], b1T=np.ascontiguousarray(f(b1)[0].reshape(NEXP, 32, 128).transpose(2, 0, 1)),
        w2=f(w2)[0], b2=f(b2)[0],
    )
    shared.update(_consts())
    maps = []
    for b in range(8):
        m = dict(shared)
        m["x"] = np.ascontiguousarray(x[b])
        m["cT"] = np.ascontiguousarray(c[b].reshape(16, 128).T)
        maps.append(m)
    return maps


def kernel(**inputs):
    in_maps = make_in_maps(**inputs)
    nc = build(dbg=False)
    res = run_bass_kernel_spmd(nc, in_maps, core_ids=list(range(8)))
    return np.stack([np.asarray(r["out"], dtype=np.float32) for r in res.results], axis=0)
```
